# Optimizing a Trainium2 kernel written in Bass

```python
import math
import jax
import jax.numpy as jnp
from jax import lax
import numpy as np

D_MODEL = 4096
BATCH = 4
SEQ = 4096
DEPTH = 4

HEAD_DIM = 128
SB_HEADS = 16
SB_WIDTH = SB_HEADS * HEAD_DIM
HG_HEADS = 16
HG_DK = 128
HG_DV = 128
HG_WIDTH = HG_HEADS * HG_DK
EVEN_IN = 3 * SB_WIDTH + 4 * HG_WIDTH
EVEN_MIX = SB_WIDTH + HG_HEADS * HG_DV
Q_BLOCK = 128
HG_CHUNK = 64
F_MIN = 1e-6
NSA_HEADS = 32
NSA_KV_GROUPS = 4
NSA_HPG = NSA_HEADS // NSA_KV_GROUPS
NSA_WIDTH = NSA_HEADS * HEAD_DIM
NSA_KV_WIDTH = NSA_KV_GROUPS * HEAD_DIM
ODD_IN = NSA_WIDTH + 6 * NSA_KV_WIDTH + 3 * NSA_HEADS
CMP_LEN = 32
CMP_STRIDE = 16
SLC_BLOCK = 64
N_SELECT = 16
WINDOW = 512
NSA_Q_BLOCK = 32
REL_BUCKETS = 32
REL_MAX_DIST = 128
N_EXPERTS = 32
TOP_K = 4
EXPERT_FF = 384
SWIGLU_LIMIT = 7.0
SWIGLU_ALPHA = 1.702
DN_ALPHA = (2 * DEPTH) ** 0.25
DN_BETA = (8 * DEPTH) ** -0.25
LN_EPS = 1e-5
NEG_INF = -1e30
FORCED = 1e9
N_EVEN = (DEPTH + 1) // 2
N_ODD = DEPTH // 2

kernel_name = "hybrid_sb_hgrn2_nsa_moe_deepnorm"

F32 = jnp.float32


def layer_norm(x, g, b):
    xf = x.astype(F32)
    mu = jnp.mean(xf, -1, keepdims=True)
    var = jnp.mean(jnp.square(xf - mu), -1, keepdims=True)
    return (xf - mu) * lax.rsqrt(var + LN_EPS) * g.astype(F32) + b.astype(F32)


def rms_norm(x, g):
    xf = x.astype(F32)
    return xf * lax.rsqrt(jnp.mean(xf * xf, -1, keepdims=True) + LN_EPS) * g.astype(F32)


def rel_bucket(dist):
    dist = jnp.maximum(dist, 0)
    max_exact = REL_BUCKETS // 2
    ratio = jnp.log(jnp.maximum(dist, max_exact).astype(F32) / max_exact) / math.log(REL_MAX_DIST / max_exact)
    large = jnp.minimum(max_exact + (ratio * (REL_BUCKETS - max_exact)).astype(jnp.int32), REL_BUCKETS - 1)
    return jnp.where(dist < max_exact, dist, large)


def stick_breaking_attention(q, k, v):
    b, t, h, d = q.shape
    nblk = t // Q_BLOCK
    scale = d ** -0.5
    qb = jnp.moveaxis(q.reshape(b, nblk, Q_BLOCK, h, d), 1, 0)
    kpos = jnp.arange(t)

    def block(args):
        qi, i = args
        z = jnp.einsum('bqhd,bkhd->bhqk', qi, k, preferred_element_type=F32) * scale
        qpos = i * Q_BLOCK + jnp.arange(Q_BLOCK)
        causal = kpos[None, :] < qpos[:, None]
        log_keep = jnp.where(causal, jax.nn.log_sigmoid(-z), 0.0)
        between = lax.cumsum(log_keep, axis=3, reverse=True) - log_keep
        w = jnp.where(causal, jnp.exp(jax.nn.log_sigmoid(z) + between), 0.0)
        return jnp.einsum('bhqk,bkhd->bqhd', w.astype(v.dtype), v)

    out = lax.map(block, (qb, jnp.arange(nblk)))
    return jnp.moveaxis(out, 0, 1).reshape(b, t, h, d)


def hgrn2_recurrence(q, k, v, log_f):
    b, t, h, dk = q.shape
    dv = v.shape[-1]
    n = t // HG_CHUNK

    def to_chunks(a):
        a = a.astype(F32).reshape(b, n, HG_CHUNK, h, a.shape[-1])
        return jnp.moveaxis(a, 1, 0).transpose(0, 1, 3, 2, 4)

    causal = jnp.tril(jnp.ones((HG_CHUNK, HG_CHUNK), bool))[:, :, None]

    def step(state, inp):
        qc, kc, vc, gc = inp
        cum = jnp.cumsum(gc, axis=2)
        diff = cum[:, :, :, None, :] - cum[:, :, None, :, :]
        decay = jnp.where(causal, jnp.exp(jnp.minimum(diff, 0.0)), 0.0)
        scores = jnp.einsum('bhtd,bhsd,bhtsd->bhts', qc, kc, decay)
        o = (jnp.einsum('bhts,bhse->bhte', scores, vc)
             + jnp.einsum('bhtd,bhde->bhte', qc * jnp.exp(cum), state))
        last = cum[:, :, -1, :]
        state = (jnp.exp(last)[..., None] * state
                 + jnp.einsum('bhsd,bhse->bhde', kc * jnp.exp(last[:, :, None, :] - cum), vc))
        return state, o

    s0 = jnp.zeros((b, h, dk, dv), F32)
    _, o = lax.scan(step, s0, (to_chunks(q), to_chunks(k), to_chunks(v), to_chunks(log_f)))
    return o.transpose(1, 0, 3, 2, 4).reshape(b, t, h, dv)


def even_mixer(x, w_in, w_out, lb, norm_g):
    b, t, _ = x.shape
    proj = x @ w_in
    sp = [SB_WIDTH, 2 * SB_WIDTH, 3 * SB_WIDTH, 3 * SB_WIDTH + HG_WIDTH,
          3 * SB_WIDTH + 2 * HG_WIDTH, 3 * SB_WIDTH + 3 * HG_WIDTH]
    qa, ka, va, qh, fh, ih, gh = jnp.split(proj, sp, axis=-1)
    heads_a = lambda a: a.reshape(b, t, SB_HEADS, HEAD_DIM)
    o_a = stick_breaking_attention(heads_a(qa), heads_a(ka), heads_a(va))
    f = lb + (1.0 - lb) * jax.nn.sigmoid(fh.astype(F32))
    log_f = jnp.log(jnp.maximum(f, F_MIN))
    k_in = 1.0 - f
    heads_b = lambda a: a.reshape(b, t, HG_HEADS, a.shape[-1] // HG_HEADS)
    o_b = hgrn2_recurrence(heads_b(jax.nn.silu(qh)), heads_b(k_in), heads_b(ih), heads_b(log_f))
    o_b = rms_norm(o_b, norm_g) * heads_b(jax.nn.silu(gh.astype(F32)))
    mixed = jnp.concatenate([o_a.reshape(b, t, SB_WIDTH).astype(F32), o_b.reshape(b, t, -1)], axis=-1)
    return (mixed.astype(x.dtype) @ w_out).astype(x.dtype)


def compress_blocks(a, idx, w1, w2, pos):
    blk = a[:, idx] + pos[:, None, :]
    b, n, l, g, d = blk.shape
    flat = jnp.moveaxis(blk, 3, 2).reshape(b, n, g, l * d)
    return jax.nn.silu(flat @ w1) @ w2


def nsa_mixer(x, w_in, w_out, ck_w1, ck_w2, ck_pos, cv_w1, cv_w2, cv_pos, rel_bias):
    b, t, _ = x.shape
    G, R, Dh = NSA_KV_GROUPS, NSA_HPG, HEAD_DIM
    proj = x @ w_in
    sp = [NSA_WIDTH + i * NSA_KV_WIDTH for i in range(7)]
    q, kc, vc, ks, vs, kw, vw, gates = jnp.split(proj, sp, axis=-1)
    kv = lambda a: a.reshape(b, t, G, Dh)
    q = q.reshape(b, t, G, R, Dh)
    gates = jax.nn.sigmoid(gates.astype(F32)).reshape(b, t, G, R, 3)
    n_cmp = (t - CMP_LEN) // CMP_STRIDE + 1
    cmp_start = jnp.arange(n_cmp) * CMP_STRIDE
    cmp_idx = cmp_start[:, None] + jnp.arange(CMP_LEN)[None, :]
    cmp_end = cmp_start + CMP_LEN - 1
    k_cmp = compress_blocks(kv(kc), cmp_idx, ck_w1, ck_w2, ck_pos)
    v_cmp = compress_blocks(kv(vc), cmp_idx, cv_w1, cv_w2, cv_pos)
    n_slc = t // SLC_BLOCK
    n_sel = min(N_SELECT, n_slc)
    k_blk = kv(ks).reshape(b, n_slc, SLC_BLOCK, G, Dh).transpose(0, 3, 1, 2, 4)
    v_blk = kv(vs).reshape(b, n_slc, SLC_BLOCK, G, Dh).transpose(0, 3, 1, 2, 4)
    slc_start = jnp.arange(n_slc) * SLC_BLOCK
    overlap = ((cmp_start[:, None] < slc_start[None, :] + SLC_BLOCK)
               & (cmp_end[:, None] >= slc_start[None, :])).astype(F32)
    kw_pad = jnp.pad(kv(kw), ((0, 0), (WINDOW, 0), (0, 0), (0, 0)))
    vw_pad = jnp.pad(kv(vw), ((0, 0), (WINDOW, 0), (0, 0), (0, 0)))
    bias_tab = rel_bias.astype(F32).reshape(REL_BUCKETS, G, R)
    bias_tab_g = bias_tab.transpose(1, 0, 2)
    n_qb = t // NSA_Q_BLOCK
    q_blocks = jnp.moveaxis(q.reshape(b, n_qb, NSA_Q_BLOCK, G, R, Dh), 1, 0)
    g_blocks = jnp.moveaxis(gates.reshape(b, n_qb, NSA_Q_BLOCK, G, R, 3), 1, 0)
    scale = Dh ** -0.5
    b_idx = jnp.arange(b)[:, None, None, None]
    g_idx = jnp.arange(G)[None, :, None, None]

    def block(args):
        qi, gb, i = args
        qpos = i * NSA_Q_BLOCK + jnp.arange(NSA_Q_BLOCK)
        s_c = jnp.einsum('bqgrd,bngd->bgrqn', qi, k_cmp, preferred_element_type=F32) * scale
        s_c = s_c + jnp.transpose(bias_tab[rel_bucket(qpos[:, None] - cmp_end[None, :])], (2, 3, 0, 1))
        m_c = cmp_end[None, :] <= qpos[:, None]
        p_c = jax.nn.softmax(jnp.where(m_c, s_c, NEG_INF), axis=-1) * m_c
        o_c = jnp.einsum('bgrqn,bngd->bqgrd', p_c.astype(v_cmp.dtype), v_cmp)
        imp = jnp.einsum('bgrqn,nm->bgqm', p_c, overlap)
        q_blk = qpos // SLC_BLOCK
        j = jnp.arange(n_slc)[None, :]
        valid = j <= q_blk[:, None]
        forced = (j == 0) | (j == q_blk[:, None]) | (j == q_blk[:, None] - 1)
        imp = jnp.where(valid & forced, FORCED, jnp.where(valid, imp, NEG_INF))
        top_val, top_idx = lax.top_k(imp, n_sel)
        k_sel = k_blk[b_idx, g_idx, top_idx]
        v_sel = v_blk[b_idx, g_idx, top_idx]
        tok = top_idx[..., None] * SLC_BLOCK + jnp.arange(SLC_BLOCK)
        m_s = (tok <= qpos[:, None, None]) & (top_val > 0.5 * NEG_INF)[..., None]
        s_s = jnp.einsum('bqgrd,bgqnkd->bgrqnk', qi, k_sel, preferred_element_type=F32) * scale
        bias_s = bias_tab_g[g_idx[..., None], rel_bucket(qpos[:, None, None] - tok)]
        s_s = jnp.where(m_s[:, :, None], s_s + jnp.moveaxis(bias_s, -1, 2), NEG_INF)
        p_s = jax.nn.softmax(s_s.reshape(b, G, R, NSA_Q_BLOCK, -1), axis=-1).reshape(s_s.shape)
        o_s = jnp.einsum('bgrqnk,bgqnkd->bqgrd', p_s.astype(v_sel.dtype), v_sel)
        kpos = i * NSA_Q_BLOCK - WINDOW + jnp.arange(WINDOW + NSA_Q_BLOCK)
        k_win = lax.dynamic_slice_in_dim(kw_pad, i * NSA_Q_BLOCK, WINDOW + NSA_Q_BLOCK, axis=1)
        v_win = lax.dynamic_slice_in_dim(vw_pad, i * NSA_Q_BLOCK, WINDOW + NSA_Q_BLOCK, axis=1)
        dist = qpos[:, None] - kpos[None, :]
        m_w = (dist >= 0) & (dist < WINDOW) & (kpos[None, :] >= 0)
        s_w = jnp.einsum('bqgrd,bkgd->bgrqk', qi, k_win, preferred_element_type=F32) * scale
        s_w = s_w + jnp.transpose(bias_tab[rel_bucket(dist)], (2, 3, 0, 1))
        p_w = jax.nn.softmax(jnp.where(m_w, s_w, NEG_INF), axis=-1)
        o_w = jnp.einsum('bgrqk,bkgd->bqgrd', p_w.astype(v_win.dtype), v_win)
        return gb[..., 0:1] * o_c + gb[..., 1:2] * o_s + gb[..., 2:3] * o_w

    out = lax.map(block, (q_blocks, g_blocks, jnp.arange(n_qb)))
    out = jnp.moveaxis(out, 0, 1).reshape(b, t, NSA_WIDTH)
    return (out.astype(x.dtype) @ w_out).astype(x.dtype)


def moe_ffn(x, w_router, b_router, w_gu, b_gu, w_down, b_down):
    b, t, d = x.shape
    xt = x.reshape(b * t, d)
    logits = (xt @ w_router).astype(F32) + b_router.astype(F32)
    top_val, top_idx = lax.top_k(logits, TOP_K)
    top_w = jax.nn.softmax(top_val, axis=-1)
    gate = jnp.sum(jax.nn.one_hot(top_idx, N_EXPERTS, dtype=F32) * top_w[..., None], axis=1)
    out = jnp.zeros((b * t, d), F32)
    for e in range(N_EXPERTS):
        hgu = (xt @ w_gu[e] + b_gu[e]).astype(F32)
        glu = jnp.minimum(hgu[:, 0::2], SWIGLU_LIMIT)
        lin = jnp.clip(hgu[:, 1::2], -SWIGLU_LIMIT, SWIGLU_LIMIT)
        act = glu * jax.nn.sigmoid(SWIGLU_ALPHA * glu) * (lin + 1.0)
        out = out + gate[:, e:e + 1] * (act.astype(x.dtype) @ w_down[e] + b_down[e]).astype(F32)
    return out.astype(x.dtype).reshape(b, t, d)


def setup_inputs(seed: int = 0) -> dict:
    key = jax.random.key(seed)
    ks = jax.random.split(key, 24)

    def normal(k, shape, scale):
        return jax.random.normal(k, shape, F32) * scale

    L = CMP_LEN
    return {
        "x": normal(ks[0], (BATCH, SEQ, D_MODEL), 1.0),
        "ln1_g": 1.0 + normal(ks[1], (DEPTH, D_MODEL), 0.02),
        "ln1_b": normal(ks[2], (DEPTH, D_MODEL), 0.02),
        "ln2_g": 1.0 + normal(ks[3], (DEPTH, D_MODEL), 0.02),
        "ln2_b": normal(ks[4], (DEPTH, D_MODEL), 0.02),
        "ev_w_in": normal(ks[5], (N_EVEN, D_MODEL, EVEN_IN), D_MODEL ** -0.5),
        "ev_w_out": normal(ks[6], (N_EVEN, EVEN_MIX, D_MODEL), DN_BETA * EVEN_MIX ** -0.5),
        "hg_lb_raw": normal(ks[7], (DEPTH, HG_WIDTH), 0.5),
        "hg_norm_g": 1.0 + normal(ks[8], (N_EVEN, HG_DV), 0.02),
        "od_w_in": normal(ks[9], (N_ODD, D_MODEL, ODD_IN), D_MODEL ** -0.5),
        "od_w_out": normal(ks[10], (N_ODD, NSA_WIDTH, D_MODEL), DN_BETA * NSA_WIDTH ** -0.5),
        "cmp_k_w1": normal(ks[11], (N_ODD, L * HEAD_DIM, HEAD_DIM), (L * HEAD_DIM) ** -0.5),
        "cmp_k_w2": normal(ks[12], (N_ODD, HEAD_DIM, HEAD_DIM), HEAD_DIM ** -0.5),
        "cmp_k_pos": normal(ks[13], (N_ODD, L, HEAD_DIM), 0.1),
        "cmp_v_w1": normal(ks[14], (N_ODD, L * HEAD_DIM, HEAD_DIM), (L * HEAD_DIM) ** -0.5),
        "cmp_v_w2": normal(ks[15], (N_ODD, HEAD_DIM, HEAD_DIM), HEAD_DIM ** -0.5),
        "cmp_v_pos": normal(ks[16], (N_ODD, L, HEAD_DIM), 0.1),
        "rel_bias": normal(ks[17], (REL_BUCKETS, NSA_HEADS), 0.2),
        "router_w": normal(ks[18], (DEPTH, D_MODEL, N_EXPERTS), D_MODEL ** -0.5),
        "router_b": normal(ks[19], (DEPTH, N_EXPERTS), 0.01),
        "exp_w_gu": normal(ks[20], (DEPTH, N_EXPERTS, D_MODEL, 2 * EXPERT_FF), D_MODEL ** -0.5),
        "exp_b_gu": normal(ks[21], (DEPTH, N_EXPERTS, 2 * EXPERT_FF), 0.02),
        "exp_w_down": normal(ks[22], (DEPTH, N_EXPERTS, EXPERT_FF, D_MODEL), DN_BETA * EXPERT_FF ** -0.5),
        "exp_b_down": normal(ks[23], (DEPTH, N_EXPERTS, D_MODEL), 0.02),
    }


def reference(x, ln1_g, ln1_b, ln2_g, ln2_b, ev_w_in, ev_w_out, hg_lb_raw, hg_norm_g,
              od_w_in, od_w_out, cmp_k_w1, cmp_k_w2, cmp_k_pos, cmp_v_w1, cmp_v_w2, cmp_v_pos,
              rel_bias, router_w, router_b, exp_w_gu, exp_b_gu, exp_w_down, exp_b_down):
    lb_soft = jax.nn.softmax(hg_lb_raw.astype(F32), axis=0)
    lower_bounds = jnp.cumsum(lb_soft, axis=0) - lb_soft[0]
    h = x
    for layer in range(DEPTH):
        if layer % 2 == 0:
            e = layer // 2
            mix = even_mixer(h, ev_w_in[e], ev_w_out[e], lower_bounds[layer], hg_norm_g[e])
        else:
            o = layer // 2
            mix = nsa_mixer(h, od_w_in[o], od_w_out[o], cmp_k_w1[o], cmp_k_w2[o], cmp_k_pos[o],
                            cmp_v_w1[o], cmp_v_w2[o], cmp_v_pos[o], rel_bias)
        h = layer_norm(DN_ALPHA * h + mix, ln1_g[layer], ln1_b[layer]).astype(x.dtype)
        ffn = moe_ffn(h, router_w[layer], router_b[layer], exp_w_gu[layer], exp_b_gu[layer],
                      exp_w_down[layer], exp_b_down[layer])
        h = layer_norm(DN_ALPHA * h + ffn, ln2_g[layer], ln2_b[layer]).astype(x.dtype)
    return h
```

```python
import math
from contextlib import ExitStack
import numpy as np
import concourse.bass as bass
import concourse.mybir as mybir
from concourse.bass_utils import run_bass_kernel_spmd

F32 = mybir.dt.float32
BF16 = mybir.dt.bfloat16
AF = mybir.ActivationFunctionType
ALU = mybir.AluOpType
AX = mybir.AxisListType


class V:
    def __init__(self, buf, ap):
        self.buf = buf
        self.ap = ap

    def __getitem__(self, k):
        return V(self.buf, self.ap[k])

    def re(self, pat, **kw):
        return V(self.buf, self.ap.rearrange(pat, **kw))

    def bc(self, shape):
        return V(self.buf, self.ap.to_broadcast(list(shape)))

    def un(self, ax):
        return V(self.buf, self.ap.unsqueeze(ax))


class Buf:
    def __init__(self, t, name):
        self.t = t
        self.name = name
        self.w = None
        self.r = {}
        self.aliases = []

    def __getitem__(self, k):
        return V(self, self.t[k])


class SubBuf(Buf):
    def __init__(self, ap, name):
        Buf.__init__(self, None, name)
        self.base = ap

    def __getitem__(self, k):
        return V(self, self.base[k])


def carve(region, off, shape, dt_size, name):
    n = int(np.prod(shape[1:]))
    ap = region.t[0:shape[0], off:off + n]
    if len(shape) == 3:
        ap = ap.rearrange("p (a b) -> p a b", a=shape[1])
    return SubBuf(ap, name), off + n


class Prog:
    ENG = ("pe", "act", "dve", "pool", "sp")
    NDSEM = 96

    def __init__(self):
        self.nc = bass.Bass("TRN2", target_bir_lowering=False)
        nc = self.nc
        self.e = {"pe": nc.tensor, "act": nc.scalar, "dve": nc.vector, "pool": nc.gpsimd, "sp": nc.sync}
        self.sem = {k: nc.alloc_semaphore(f"S_{k}") for k in self.ENG}
        self.cnt = {k: 0 for k in self.ENG}
        self.waited = {k: {} for k in self.ENG}
        self.pend = {k: {} for k in self.ENG}
        self.fold = True
        self.fold_dma = True
        self.nfold = 1
        self.fold_engs = ('dve', 'act', 'pool', 'pe')
        self.same_sync = True
        self.nsame = 0
        self.dsem = [[nc.alloc_semaphore(f"D{i}"), 0] for i in range(self.NDSEM)]
        self.dnext = 0
        self.nbuf = 0
        self.ninst = 0

    def sb(self, st, shape, dt=F32, name=None):
        self.nbuf += 1
        name = name or f"sb{self.nbuf}"
        return Buf(st.enter_context(self.nc.sbuf_tensor(name, list(shape), dt)), name)

    def ps(self, st, shape, dt=F32, name=None):
        self.nbuf += 1
        name = name or f"ps{self.nbuf}"
        return Buf(st.enter_context(self.nc.psum_tensor(name, list(shape), dt)), name)

    def dram(self, name, shape, dt=F32, kind="Internal"):
        return Buf(self.nc.dram_tensor(name, list(shape), dt, kind=kind), name)

    def _wait(self, eng, tok):
        if tok is None:
            return
        kind, key, val = tok
        if kind == "eng" and key == eng and (eng == "pe" or not self.same_sync):
            return
        if kind == "eng" and key == eng:
            self.nsame += 1
        wk = (kind, key)
        if self.waited[eng].get(wk, 0) >= val:
            return
        self.waited[eng][wk] = val
        self.pend[eng][wk] = val

    def _flush(self, eng, keep_last=False):
        items = list(self.pend[eng].items())
        self.pend[eng] = {}
        held = None
        if keep_last and items:
            held = items[-self.nfold:]
            items = items[:-self.nfold]
        for (kind, key), val in items:
            sem = self.sem[key] if kind == "eng" else self.dsem[key][0]
            self.e[eng].wait_ge(sem, val)
            self.ninst += 1
            self.nwait = getattr(self, "nwait", 0) + 1
        return held

    def _deps(self, eng, reads, writes):
        for b in reads:
            self._wait(eng, b.w)
        for b in writes:
            for x in [b] + b.aliases:
                self._wait(eng, x.w)
                for (kind, key), val in x.r.items():
                    self._wait(eng, (kind, key, val))

    def _record(self, tok, reads, writes):
        for b in reads:
            b.r[(tok[0], tok[1])] = tok[2]
        for b in writes:
            b.w = tok
            b.r = {}

    def op(self, eng, fn, reads=(), writes=()):
        self._deps(eng, reads, writes)
        held = self._flush(eng, keep_last=(self.fold and eng in self.fold_engs))
        ins = fn(self.e[eng])
        for (kind, key), val in (held or []):
            ins._wait_ge(self.sem[key] if kind == "eng" else self.dsem[key][0], val)
        self.cnt[eng] += 1
        ins.then_inc(self.sem[eng], 1)
        self.ninst += 1
        self._record(("eng", eng, self.cnt[eng]), reads, writes)
        return ins

    def dma(self, out, in_, eng="sp", **kw):
        i = self.dnext
        self.dnext = (self.dnext + 1) % self.NDSEM
        self._wait(eng, ("dma", i, self.dsem[i][1]))
        self._deps(eng, [in_.buf], [out.buf])
        held = self._flush(eng, keep_last=self.fold_dma)
        ins = self.e[eng].dma_start(out=out.ap, in_=in_.ap, **kw)
        for (kind, key), val in (held or []):
            ins._wait_ge(self.sem[key] if kind == "eng" else self.dsem[key][0], val)
        self.dsem[i][1] += 16
        ins.then_inc(self.dsem[i][0], 16)
        self.ninst += 1
        self.ndma = getattr(self, 'ndma', 0) + 1
        self._record(("dma", i, self.dsem[i][1]), [in_.buf], [out.buf])
        return ins

    def barrier(self):
        for e in self.ENG:
            for e2 in self.ENG:
                if e2 != e:
                    self._wait(e, ("eng", e2, self.cnt[e2]))
            for i in range(self.NDSEM):
                self._wait(e, ("dma", i, self.dsem[i][1]))
            self._flush(e)

    def mm(self, out, lhsT, rhs, start=True, stop=True):
        return self.op("pe", lambda E: E.matmul(out.ap, lhsT=lhsT.ap, rhs=rhs.ap, start=start, stop=stop),
                       reads=[lhsT.buf, rhs.buf], writes=[out.buf])

    def tr(self, out, in_, ident):
        return self.op("pe", lambda E: E.transpose(out.ap, in_.ap, ident.ap), reads=[in_.buf, ident.buf], writes=[out.buf])

    def act(self, out, in_, func, scale=None, bias=None, eng="act"):
        kw = {}
        rd = [in_.buf]
        if scale is not None:
            if isinstance(scale, V):
                kw["scale"] = scale.ap
                rd.append(scale.buf)
            else:
                kw["scale"] = float(scale)
        if bias is not None:
            if isinstance(bias, V):
                kw["bias"] = bias.ap
                rd.append(bias.buf)
            else:
                kw["bias"] = float(bias)
        return self.op(eng, lambda E: E.activation(out=out.ap, in_=in_.ap, func=func, **kw), reads=rd, writes=[out.buf])

    def tt(self, out, in0, in1, op, eng="dve"):
        return self.op(eng, lambda E: E.tensor_tensor(out=out.ap, in0=in0.ap, in1=in1.ap, op=op),
                       reads=[in0.buf, in1.buf], writes=[out.buf])

    def ts(self, out, in0, s1, op0, s2=None, op1=None, eng="dve"):
        rd = [in0.buf]
        a1 = s1
        if isinstance(s1, V):
            rd.append(s1.buf)
            a1 = s1.ap
        a2 = s2
        if isinstance(s2, V):
            rd.append(s2.buf)
            a2 = s2.ap
        if op1 is None:
            return self.op(eng, lambda E: E.tensor_scalar(out=out.ap, in0=in0.ap, scalar1=a1, scalar2=None, op0=op0),
                           reads=rd, writes=[out.buf])
        return self.op(eng, lambda E: E.tensor_scalar(out=out.ap, in0=in0.ap, scalar1=a1, scalar2=a2, op0=op0, op1=op1),
                       reads=rd, writes=[out.buf])

    def stt(self, out, in0, scalar, in1, op0, op1):
        rd = [in0.buf, in1.buf]
        a = scalar
        if isinstance(scalar, V):
            rd.append(scalar.buf)
            a = scalar.ap
        return self.op("dve", lambda E: E.scalar_tensor_tensor(out=out.ap, in0=in0.ap, scalar=a, in1=in1.ap, op0=op0, op1=op1),
                       reads=rd, writes=[out.buf])

    def copy(self, out, in_, eng="dve"):
        if eng == "act":
            return self.act(out, in_, AF.Copy)
        return self.op(eng, lambda E: E.tensor_copy(out=out.ap, in_=in_.ap), reads=[in_.buf], writes=[out.buf])

    def memset(self, out, val, eng="dve"):
        return self.op(eng, lambda E: E.memset(out.ap, float(val)), writes=[out.buf])

    def recip(self, out, in_):
        return self.op("dve", lambda E: E.reciprocal(out=out.ap, in_=in_.ap), reads=[in_.buf], writes=[out.buf])


HD = 128
SB_H = 16
HG_H = 16
EVEN_IN = 14336
NSA_H = 32
NSA_G = 4
ODD_IN = 7264
NE = 32
EFF = 384
LIM = 7.0
SW_ALPHA = 1.702
LN_EPS = 1e-5
F_MIN = 1e-6
HG_C = 64
TB = 512


class Cfg:
    def __init__(self, D=4096, T=4096, NB=4, NCORE=1, DEPTH=4):
        self.D, self.T, self.NB, self.NCORE, self.DEPTH = D, T, NB, NCORE, DEPTH
        self.KC = D // 128
        self.NTOK = NB * T
        self.NBLK = self.NTOK // TB
        self.alpha = (2 * DEPTH) ** 0.25


def fm_layout(w):
    K, M = w.shape
    return np.ascontiguousarray(w.reshape(K // 128, 128, M // 128, 128).transpose(2, 1, 0, 3))


def tm_layout(w, nw=256):
    K, N = w.shape
    return np.ascontiguousarray(w.reshape(K // 128, 128, N // nw, nw).transpose(2, 1, 0, 3))


def pvec(v):
    return np.ascontiguousarray(v.reshape(-1, 128).T)


class Builder:
    def __init__(self, cfg, layer_kinds=None):
        self.c = cfg
        self.p = Prog()
        self.shapes = {}
        self.kinds = layer_kinds or ["even" if l % 2 == 0 else "odd" for l in range(cfg.DEPTH)]
        self.ncast = 0

    def inp(self, name, shape, dt=F32):
        b = self.p.dram(name, shape, dt, kind="ExternalInput")
        self.shapes[name] = tuple(shape)
        return b

    def scratch(self, name, shape, dt):
        return self.p.dram(name, shape, dt)

    def cast_w(self, st, src, shape):
        p = self.p
        self.ncast += 1
        dst = self.scratch(src.name + "_bf", shape, BF16)
        n = int(np.prod(shape))
        per = n // 128
        names = " ".join(f"d{i}" for i in range(len(shape)))
        sflat = src[:].re(f"{names} -> ({names})").re("(p f) -> p f", p=128)
        dflat = dst[:].re(f"{names} -> ({names})").re("(p f) -> p f", p=128)
        F = 4096
        off = 0
        i = 0
        while off < per:
            w = min(F, per - off)
            a = self.cf32[i % 2]
            b = self.cb16[i % 2]
            p.dma(a[:, 0:w], sflat[:, off:off + w])
            p.copy(b[:, 0:w], a[:, 0:w], eng="pool" if i % 2 == 0 else "act")
            p.dma(dflat[:, off:off + w], b[:, 0:w])
            off += w
            i += 1
        return dst

    def build(self):
        c, p = self.c, self.p
        D, T, NB, KC, NTOK, NBLK = c.D, c.T, c.NB, c.KC, c.NTOK, c.NBLK
        self.glob = ExitStack()
        g = self.glob
        self.ps = [p.ps(g, [128, 512], F32, name=f"psb{i}") for i in range(7)]
        self.psb = p.ps(g, [128, 1024], BF16, name="psbf")
        self.c_ident32 = self.inp("c_ident32", [128, 128])
        self.c_tri64 = self.inp("c_tri64", [64, 64])
        self.c_rmask = self.inp("c_rmask", [128, TB])
        self.c_sbmask = self.inp("c_sbmask", [4, 128, TB])
        self.c_triincl = self.inp("c_triincl", [128, 128])
        self.ident32 = p.sb(g, [128, 128], F32, name="ident32")
        self.ident16 = p.sb(g, [128, 128], BF16, name="ident16")
        self.onesD = p.sb(g, [128, 128], F32, name="onesD")
        self.ones128 = p.sb(g, [128, 128], F32, name="ones128")
        self.negones16 = p.sb(g, [128, 128], BF16, name="negones16")
        self.eps_t = p.sb(g, [128, 1], F32, name="eps_t")
        p.dma(self.ident32[:], self.c_ident32[:])
        p.copy(self.ident16[:], self.ident32[:])
        p.memset(self.onesD[:], 1.0 / D)
        p.memset(self.ones128[:], 1.0 / 128)
        p.memset(self.negones16[:], -1.0)
        p.memset(self.eps_t[:], LN_EPS)
        self.H32 = [self.scratch(f"H32_{i}", [KC, 128, NTOK], F32) for i in range(2)]
        self.H16 = [self.scratch(f"H16_{i}", [KC, 128, NTOK], BF16) for i in range(2)]
        self.zT = [self.scratch(f"zT_{i}", [KC, 128, TB], F32) for i in range(2)]
        self.mixedT = self.scratch("mixedT", [32, 128, NTOK], BF16)
        xT = self.inp("xT", [KC, 128, NTOK])
        outT = self.p.dram("outT", [KC, 128, NTOK], F32, kind="ExternalOutput")
        self.shapes_out = ("outT", (KC, 128, NTOK))

        with ExitStack() as st:
            a = [p.sb(st, [128, 8, TB], F32) for _ in range(2)]
            b16 = [p.sb(st, [128, 8, TB], BF16) for _ in range(2)]
            i = 0
            for b in range(NBLK):
                for gq in range(0, KC, 8):
                    n = min(8, KC - gq)
                    sl = slice(b * TB, (b + 1) * TB)
                    p.dma(a[i % 2][:, 0:n, :], xT[gq:gq + n, :, sl].re("c p t -> p c t"))
                    p.dma(self.H32[0][gq:gq + n, :, sl].re("c p t -> p c t"), a[i % 2][:, 0:n, :])
                    p.copy(b16[i % 2][:, 0:n, :], a[i % 2][:, 0:n, :], eng="pool" if i % 2 else "act")
                    p.dma(self.H16[0][gq:gq + n, :, sl].re("c p t -> p c t"), b16[i % 2][:, 0:n, :])
                    i += 1
        p.barrier()

        self.prep_lb()

        for l in range(c.DEPTH):
            kind = self.kinds[l]
            if kind == "even":
                self.even_layer(l)
            elif kind == "odd":
                self.odd_layer(l)
            self.moe_layer(l)

        with ExitStack() as st:
            a = [p.sb(st, [128, 8, TB], F32) for _ in range(2)]
            i = 0
            for b in range(NBLK):
                for gq in range(0, KC, 8):
                    n = min(8, KC - gq)
                    sl = slice(b * TB, (b + 1) * TB)
                    p.dma(a[i % 2][:, 0:n, :], self.H32[0][gq:gq + n, :, sl].re("c p t -> p c t"))
                    p.dma(outT[gq:gq + n, :, sl].re("c p t -> p c t"), a[i % 2][:, 0:n, :])
                    i += 1
        p.barrier()
        return self

    def prep_lb(self):
        c, p, g = self.c, self.p, self.glob
        DEPTH = c.DEPTH
        raw = self.inp("lb_raw", [128, DEPTH, 16])
        t = p.sb(g, [128, DEPTH, 16], F32, name="lb_t")
        e = p.sb(g, [128, DEPTH, 16], F32, name="lb_e")
        s = p.sb(g, [128, 16], F32, name="lb_s")
        self.lb = p.sb(g, [128, DEPTH, 16], F32, name="lb")
        self.oml = p.sb(g, [128, DEPTH, 16], F32, name="oml")
        p.dma(t[:], raw[:])
        p.act(e[:], t[:], AF.Exp)
        p.copy(s[:], e[:, 0, :])
        for l in range(1, DEPTH):
            p.tt(s[:], s[:], e[:, l, :], ALU.add)
        p.recip(s[:], s[:])
        p.memset(self.lb[:, 0, :], 0.0)
        for l in range(1, DEPTH):
            p.tt(t[:, l, :], e[:, l, :], s[:], ALU.mult)
            p.tt(self.lb[:, l, :], self.lb[:, l - 1, :], t[:, l, :], ALU.add)
        p.ts(self.oml[:], self.lb[:], -1.0, ALU.mult, 1.0, ALU.add)

    def proj_res_ln(self, st, blk, Ablk, KA, wdram, parts, extra, hin, hout, gcol, bcol, ws, hr, zt, zq, psY, psS, psQ, stat):
        c, p = self.c, self.p
        KC = c.KC
        sl = slice(blk * TB, (blk + 1) * TB)
        zT = self.zT[blk % 2]
        kp = KA // parts
        wi = 0
        for n in range(KC):
            ps = psY[n % 2]
            first = True
            for pt in range(parts):
                w = ws[wi % len(ws)]
                wi += 1
                p.dma(w[:, 0:kp * 128], wdram[n, :, pt * kp * 128:(pt + 1) * kp * 128])
                for q in range(kp):
                    last = (pt == parts - 1 and q == kp - 1 and extra is None)
                    p.mm(ps[:], w[:, q * 128:(q + 1) * 128], Ablk[:, pt * kp + q, :], start=first, stop=last)
                    first = False
            if extra is not None:
                extra(n, ps)
            h = hr[n % 2]
            p.dma(h[:], hin[n, :, sl])
            z = zt[n % 2]
            p.stt(z[:], h[:], c.alpha, ps[:], ALU.mult, ALU.add)
            p.dma(zT[n, :, :], z[:])
            p.mm(psS[:], self.onesD[:], z[:], start=(n == 0), stop=(n == KC - 1))
            q2 = zq[n % 2]
            p.act(q2[:], z[:], AF.Square)
            p.mm(psQ[:], self.onesD[:], q2[:], start=(n == 0), stop=(n == KC - 1))
        mean, rstd, nmr, tmp = stat
        p.copy(mean[:], psS[:], eng="act")
        p.tt(tmp[:], mean[:], mean[:], ALU.mult)
        p.tt(tmp[:], psQ[:], tmp[:], ALU.subtract)
        p.act(tmp[:], tmp[:], AF.Sqrt, bias=self.eps_t[:])
        p.recip(rstd[:], tmp[:])
        p.stt(nmr[:], mean[:], -1.0, rstd[:], ALU.mult, ALU.mult)
        for n in range(KC):
            z = zt[n % 2]
            p.dma(z[:], zT[n, :, :])
            q2 = zq[n % 2]
            p.tt(q2[:], z[:], rstd[:], ALU.mult)
            p.tt(q2[:], q2[:], nmr[:], ALU.add)
            h = hr[n % 2]
            p.act(h[:], q2[:], AF.Identity, scale=gcol[:, n:n + 1], bias=bcol[:, n:n + 1])
            p.dma(self.H32[hout][n, :, sl], h[:])
            h16 = self.h16t[n % 2]
            p.copy(h16[:], h[:], eng="pool")
            p.dma(self.H16[hout][n, :, sl], h16[:])

    def even_layer(self, l):
        c, p = self.c, self.p
        D, T, NB, KC, NTOK, NBLK = c.D, c.T, c.NB, c.KC, c.NTOK, c.NBLK
        e = l // 2
        BPS = T // TB
        NCH = T // HG_C
        w_fm = self.inp(f"ev_win_fm{l}", [80, 128, KC * 128])
        w_tm = self.inp(f"ev_win_tm{l}", [16, 128, KC * 256])
        w_out = self.inp(f"ev_wout{l}", [KC, 128, 32 * 128])
        ng_in = self.inp(f"hg_ng{l}", [128, 1])
        ln_in = self.inp(f"ln1_{l}", [128, 2, KC])
        qaT = self.scratch(f"qaT{l}", [NB, 16, 128, T], BF16)
        kaT = self.scratch(f"kaT{l}", [NB, 16, 128, T], BF16)
        vaTM = self.scratch(f"vaTM{l}", [NB, T, 2048], BF16)
        ihTM = self.scratch(f"ihTM{l}", [NB, T, 2048], BF16)
        qbT = self.scratch(f"qbT{l}", [NB, 16, 128, T], BF16)
        ktT = self.scratch(f"ktT{l}", [NB, 16, 128, T], BF16)
        khT = self.scratch(f"khT{l}", [NB, 16, 128, T], BF16)
        sgT = self.scratch(f"sgT{l}", [NB, 16, 128, T], BF16)
        elD = self.scratch(f"elD{l}", [NB, 16, 128, NCH], F32)
        ps = self.ps
        with ExitStack() as st:
            self.cf32 = [p.sb(st, [128, 4096], F32) for _ in range(2)]
            self.cb16 = [p.sb(st, [128, 4096], BF16) for _ in range(2)]
            wfm16 = self.cast_w(st, w_fm, [80, 128, KC * 128])
            wtm16 = self.cast_w(st, w_tm, [16, 128, KC * 256])
            wout16 = self.cast_w(st, w_out, [KC, 128, 32 * 128])
        p.barrier()
        with ExitStack() as st:
            hb = [p.sb(st, [128, KC, TB], BF16) for _ in range(2)]
            ws = [p.sb(st, [128, KC * 128], BF16) for _ in range(3)]
            wt = [p.sb(st, [128, KC * 256], BF16) for _ in range(2)]
            o16 = [p.sb(st, [128, TB], BF16) for _ in range(4)]
            f = {k: p.sb(st, [128, TB], F32, name=f"ev{l}_{k}") for k in
                 ["sq", "sg", "f", "lf", "kin", "cum", "ec", "enc", "kt"]}
            el = p.sb(st, [128, 8], F32)
            rmask = p.sb(st, [128, TB], F32)
            p.dma(rmask[:], self.c_rmask[:])
            wi = 0
            oi = 0
            for b in range(NBLK):
                s, tb = divmod(b, BPS)
                h = hb[b % 2]
                tsl = slice(tb * TB, (tb + 1) * TB)
                p.dma(h[:], self.H16[0][:, :, b * TB:(b + 1) * TB].re("c p t -> p c t"))

                def proj(mc, pst):
                    nonlocal wi
                    w = ws[wi % 3]
                    wi += 1
                    p.dma(w[:], wfm16[mc, :, :])
                    for k in range(KC):
                        p.mm(pst[:], w[:, k * 128:(k + 1) * 128], h[:, k, :], start=(k == 0), stop=(k == KC - 1))

                def out16():
                    nonlocal oi
                    o = o16[oi % 4]
                    oi += 1
                    return o
                for hd in range(16):
                    proj(hd, ps[0])
                    o = out16()
                    p.act(o[:], ps[0][:], AF.Copy, scale=HD ** -0.5)
                    p.dma(qaT[s, hd, :, tsl], o[:])
                    proj(16 + hd, ps[1])
                    o = out16()
                    p.copy(o[:], ps[1][:], eng="dve")
                    p.dma(kaT[s, hd, :, tsl], o[:])
                    proj(32 + hd, ps[2])
                    p.act(f["sq"][:], ps[2][:], AF.Silu)
                    proj(48 + hd, ps[3])
                    p.act(f["sg"][:], ps[3][:], AF.Sigmoid)
                    p.ts(f["f"][:], f["sg"][:], self.oml[:, l, hd:hd + 1], ALU.mult, self.lb[:, l, hd:hd + 1], ALU.add)
                    p.ts(f["lf"][:], f["f"][:], F_MIN, ALU.max)
                    p.act(f["lf"][:], f["lf"][:], AF.Ln)
                    p.ts(f["kin"][:], f["f"][:], -1.0, ALU.mult, 1.0, ALU.add)
                    p.op("dve", lambda E: E.tensor_tensor_scan(out=f["cum"][:].ap, data0=rmask[:].ap, data1=f["lf"][:].ap,
                                                               initial=0.0, op0=ALU.mult, op1=ALU.add),
                         reads=[rmask, f["lf"]], writes=[f["cum"]])
                    p.act(f["ec"][:], f["cum"][:], AF.Exp)
                    p.act(f["enc"][:], f["cum"][:], AF.Exp, scale=-1.0)
                    o = out16()
                    p.tt(o[:], f["sq"][:], f["ec"][:], ALU.mult)
                    p.dma(qbT[s, hd, :, tsl], o[:])
                    p.tt(f["kt"][:], f["kin"][:], f["enc"][:], ALU.mult)
                    o = out16()
                    p.copy(o[:], f["kt"][:], eng="pool")
                    p.dma(ktT[s, hd, :, tsl], o[:])
                    p.copy(el[:], f["ec"][:].re("p (c k) -> p c k", k=HG_C)[:, :, HG_C - 1], eng="pool")
                    p.dma(elD[s, hd, :, tb * 8:(tb + 1) * 8], el[:])
                    o = out16()
                    p.tt(o[:].re("p (c k) -> p c k", k=HG_C), f["kt"][:].re("p (c k) -> p c k", k=HG_C),
                         el[:].un(2).bc([128, 8, HG_C]), ALU.mult)
                    p.dma(khT[s, hd, :, tsl], o[:])
                    proj(64 + hd, ps[4])
                    o = out16()
                    p.act(o[:], ps[4][:], AF.Silu)
                    p.dma(sgT[s, hd, :, tsl], o[:])
                for sb_ in range(16):
                    w = wt[sb_ % 2]
                    p.dma(w[:], wtm16[sb_, :, :])
                    dst = vaTM if sb_ < 8 else ihTM
                    col = (sb_ % 8) * 256
                    for j2 in range(2):
                        pst = ps[5 + (j2 % 2)]
                        for jj in range(2):
                            j = j2 * 2 + jj
                            for k in range(KC):
                                p.mm(pst[:, jj * 256:(jj + 1) * 256], h[:, k, j * 128:(j + 1) * 128],
                                     w[:, k * 256:(k + 1) * 256], start=(k == 0), stop=(k == KC - 1))
                        o = out16()
                        p.copy(o[:], pst[:], eng="act" if j2 else "dve")
                        r0 = tb * TB + j2 * 256
                        p.dma(dst[s, r0:r0 + 256, col:col + 256].re("(j p) n -> p j n", p=128),
                              o[:].re("p (j n) -> p j n", j=2))
        p.barrier()
        self.sb_attention(l, qaT, kaT, vaTM)
        p.barrier()
        self.hgrn(l, qbT, ktT, khT, sgT, elD, ihTM, ng_in)
        p.barrier()
        self.out_proj_ln(l, wout16, ln_in)

    def out_proj_ln(self, l, wout16, ln_in):
        c, p = self.c, self.p
        KC, NBLK = c.KC, c.NBLK
        ps = self.ps
        with ExitStack() as st:
            ab = [p.sb(st, [128, 32, TB], BF16) for _ in range(2)]
            ws = [p.sb(st, [128, 32 * 128], BF16) for _ in range(3)]
            hr = [p.sb(st, [128, TB], F32) for _ in range(2)]
            zt = [p.sb(st, [128, TB], F32) for _ in range(2)]
            zq = [p.sb(st, [128, TB], F32) for _ in range(2)]
            self.h16t = [p.sb(st, [128, TB], BF16) for _ in range(2)]
            stat = [p.sb(st, [128, TB], F32) for _ in range(4)]
            lnp = p.sb(st, [128, 2, KC], F32)
            p.dma(lnp[:], ln_in[:])
            for b in range(NBLK):
                a = ab[b % 2]
                p.dma(a[:], self.mixedT[:, :, b * TB:(b + 1) * TB].re("c p t -> p c t"))
                self.proj_res_ln(st, b, a, 32, wout16, 1, None, self.H32[0], 1, lnp[:, 0, :], lnp[:, 1, :],
                                 ws, hr, zt, zq, [ps[0], ps[1]], ps[2], ps[3], stat)
        p.barrier()

    def sb_attention(self, l, qaT, kaT, vaTM):
        c, p = self.c, self.p
        T, NB = c.T, c.NB
        ps = self.ps
        NT = T // TB
        with ExitStack() as st:
            kT = [p.sb(st, [128, T], BF16) for _ in range(2)]
            vv = [p.sb(st, [128, T // 128, 128], BF16) for _ in range(2)]
            qt = [p.sb(st, [128, TB], BF16) for _ in range(2)]
            e1 = [p.sb(st, [128, TB], F32) for _ in range(2)]
            sp = [p.sb(st, [128, TB], BF16) for _ in range(2)]
            ls32 = p.sb(st, [128, TB], F32)
            ls16 = p.sb(st, [128, TB], BF16)
            wT = [p.sb(st, [128, TB], BF16) for _ in range(2)]
            o16 = [p.sb(st, [128, TB], BF16) for _ in range(2)]
            msk = p.sb(st, [128, 4, TB], BF16)
            mskf = p.sb(st, [128, 4, TB], F32)
            tri = p.sb(st, [128, 128], BF16)
            trif = p.sb(st, [128, 128], F32)
            p.dma(mskf[:], self.c_sbmask[:].re("r s t -> s r t"))
            p.copy(msk[:], mskf[:])
            p.dma(trif[:], self.c_triincl[:])
            p.copy(tri[:], trif[:])
            it = 0
            pi = 0
            for s in range(NB):
                for hd in range(16):
                    k = kT[it % 2]
                    v = vv[it % 2]
                    it += 1
                    p.dma(k[:], kaT[s, hd, :, :])
                    p.dma(v[:], vaTM[s, :, hd * 128:(hd + 1) * 128].re("(c p) d -> p c d", p=128))
                    for i in range(NT):
                        q = qt[i % 2]
                        p.dma(q[:], qaT[s, hd, :, i * TB:(i + 1) * TB])
                        psO = ps[4 + (i % 2)]
                        nchunks = 4 * i + 4
                        for ci, cc in enumerate(range(nchunks - 1, -1, -1)):
                            psA = ps[pi % 2]
                            psB = ps[2 + pi % 2]
                            e = e1[pi % 2]
                            spc = sp[pi % 2]
                            w = wT[pi % 2]
                            pi += 1
                            kc_ = k[:, cc * 128:(cc + 1) * 128]
                            diag = cc >= 4 * i
                            p.mm(psA[:], kc_, q[:])
                            p.act(e[:], psA[:], AF.Exp)
                            p.act(spc[:], e[:], AF.Ln, bias=1.0)
                            if diag:
                                p.tt(spc[:], spc[:], msk[:, cc - 4 * i, :], ALU.mult, eng="pool")
                            p.mm(psB[:], kc_, q[:], start=True, stop=False)
                            p.mm(psB[:], tri[:], spc[:], start=False, stop=(ci == 0))
                            if ci > 0:
                                p.mm(psB[:], self.negones16[:], ls16[:], start=False, stop=True)
                            p.act(w[:], psB[:], AF.Exp)
                            if diag:
                                p.tt(w[:], w[:], msk[:, cc - 4 * i, :], ALU.mult, eng="pool")
                            p.mm(psO[:], v[:, cc, :], w[:], start=(ci == 0), stop=(cc == 0))
                            if cc > 0:
                                if ci == 0:
                                    p.copy(ls32[:], spc[:], eng="dve")
                                else:
                                    p.tt(ls32[:], ls32[:], spc[:], ALU.add)
                                p.copy(ls16[:], ls32[:], eng="pool")
                        o = o16[i % 2]
                        p.copy(o[:], psO[:], eng="dve")
                        p.dma(self.mixedT[hd, :, s * T + i * TB: s * T + (i + 1) * TB], o[:])

    def hgrn(self, l, qbT, ktT, khT, sgT, elD, ihTM, ng_in):
        c, p = self.c, self.p
        T, NB = c.T, c.NB
        ps = self.ps
        NT = T // TB
        NCH = T // HG_C
        G = 8
        with ExitStack() as st:
            def mk(shape, dt, n=1):
                return [[p.sb(st, shape, dt) for _ in range(n)] for _ in range(2)]
            qb = mk([128, G, TB], BF16)
            kt = mk([128, G, TB], BF16)
            kh = mk([128, G, TB], BF16)
            sg = mk([128, G, TB], BF16)
            vv = mk([64, 8, G, 128], BF16)
            el = mk([128, G, 8], F32)
            khTM = [p.sb(st, [64, 8, G, 128], BF16) for _ in range(2)]
            S32 = [p.sb(st, [128, G, 128], F32) for _ in range(2)]
            S16 = [p.sb(st, [128, G, 128], BF16) for _ in range(2)]
            stmp = p.sb(st, [128, G, 128], F32)
            sm = [p.sb(st, [64, G, 64], BF16) for _ in range(2)]
            oall = [p.sb(st, [128, G, TB], F32) for _ in range(2)]
            tri = p.sb(st, [64, 64], F32)
            ng = p.sb(st, [128, 1], F32)
            sq = p.sb(st, [128, TB], F32)
            rs = p.sb(st, [128, TB], F32)
            y1 = p.sb(st, [128, TB], F32)
            o16 = [p.sb(st, [128, TB], BF16) for _ in range(2)]
            p.dma(tri[:], self.c_tri64[:])
            p.dma(ng[:], ng_in[:])
            psS = [ps[0], ps[1]]
            psO = [ps[2], ps[3]]
            for s in range(NB):
                for gi in range(2):
                    p.memset(S32[gi][:], 0.0)
                    p.memset(S16[gi][:], 0.0)
                for i in range(NT):
                    tsl = slice(i * TB, (i + 1) * TB)
                    for gi in range(2):
                        hs = slice(gi * G, (gi + 1) * G)
                        b = 0
                        p.dma(qb[gi][b][:], qbT[s, hs, :, tsl].re("h p t -> p h t"))
                        p.dma(kt[gi][b][:], ktT[s, hs, :, tsl].re("h p t -> p h t"))
                        p.dma(kh[gi][b][:], khT[s, hs, :, tsl].re("h p t -> p h t"))
                        p.dma(sg[gi][b][:], sgT[s, hs, :, tsl].re("h p t -> p h t"))
                        p.dma(el[gi][b][:], elD[s, hs, :, i * 8:(i + 1) * 8].re("h p c -> p h c"))
                        p.dma(vv[gi][b][:], ihTM[s, tsl, gi * G * 128:(gi + 1) * G * 128].re("(c p) (h e) -> p c h e", p=64, e=128))
                        for cq in range(8):
                            for j in range(G):
                                p.tr(self.psb[0:64, j * 128:(j + 1) * 128], kh[gi][b][:, j, cq * 64:(cq + 1) * 64], self.ident16[:])
                            p.copy(khTM[gi][0:64, cq, :, :].re("p h e -> p (h e)"), self.psb[0:64, :], eng="act")
                    for cq in range(8):
                        cs = slice(cq * 64, (cq + 1) * 64)
                        for gi in range(2):
                            b = 0
                            Q, K, Vv = qb[gi][b], kt[gi][b], vv[gi][b]
                            for j in range(G):
                                p.mm(psS[gi][0:64, j * 64:(j + 1) * 64], K[:, j, cs], Q[:, j, cs])
                            p.tt(sm[gi][:], psS[gi][0:64, :].re("p (h t) -> p h t", h=G), tri[:].un(1).bc([64, G, 64]), ALU.mult)
                            for j in range(G):
                                p.mm(psO[gi][:, j * 64:(j + 1) * 64], Vv[0:64, cq, j, :], sm[gi][:, j, :], start=True, stop=False)
                                p.mm(psO[gi][:, j * 64:(j + 1) * 64], S16[gi][:, j, :], Q[:, j, cs], start=False, stop=True)
                            for j in range(G):
                                pst = ps[4 + j // 4]
                                p.mm(pst[:, (j % 4) * 128:(j % 4 + 1) * 128], khTM[gi][0:64, cq, j, :], Vv[0:64, cq, j, :])
                            p.copy(oall[gi][:, :, cs], psO[gi][:].re("p (h t) -> p h t", h=G), eng="act")
                            p.tt(stmp[:], S32[gi][:], el[gi][b][:, :, cq].un(2).bc([128, G, 128]), ALU.mult)
                            for hh in range(2):
                                p.tt(S32[gi][:, hh * 4:(hh + 1) * 4, :], stmp[:, hh * 4:(hh + 1) * 4, :],
                                     ps[4 + hh][:].re("p (h e) -> p h e", h=4), ALU.add)
                            p.copy(S16[gi][:], S32[gi][:], eng="act")
                    for gi in range(2):
                        b = 0
                        for j in range(G):
                            hd = gi * G + j
                            p.act(sq[:], oall[gi][:, j, :], AF.Square)
                            p.mm(ps[6][:], self.ones128[:], sq[:])
                            p.act(rs[:], ps[6][:], AF.Sqrt, bias=self.eps_t[:])
                            p.recip(rs[:], rs[:])
                            p.tt(y1[:], oall[gi][:, j, :], rs[:], ALU.mult)
                            o = o16[j % 2]
                            p.stt(o[:], y1[:], ng[:, 0:1], sg[gi][b][:, j, :], ALU.mult, ALU.mult)
                            p.dma(self.mixedT[16 + hd, :, s * T + i * TB: s * T + (i + 1) * TB], o[:])

    def moe_layer(self, l):
        c, p = self.c, self.p
        D, KC, NBLK = c.D, c.KC, c.NBLK
        ps = self.ps
        w_gu = self.inp(f"moe_wgu{l}", [NE * 6, 128, KC * 128])
        w_dn = self.inp(f"moe_wdn{l}", [KC, 128, 96 * 128])
        b_gu = self.inp(f"moe_bgu{l}", [128, NE, 6])
        b_dn = self.inp(f"moe_bdn{l}", [NE, D])
        w_r = self.inp(f"moe_wr{l}", [128, KC, NE])
        b_r = self.inp(f"moe_br{l}", [NE, 1])
        ln_in = self.inp(f"ln2_{l}", [128, 2, KC])
        with ExitStack() as st:
            self.cf32 = [p.sb(st, [128, 4096], F32) for _ in range(2)]
            self.cb16 = [p.sb(st, [128, 4096], BF16) for _ in range(2)]
            wgu16 = self.cast_w(st, w_gu, [NE * 6, 128, KC * 128])
            wdn16 = self.cast_w(st, w_dn, [KC, 128, 96 * 128])
        p.barrier()
        with ExitStack() as st:
            RN = max(KC * TB + 4 * KC * 128, 3 * 48 * 128)
            R = p.sb(st, [128, RN], BF16, name=f"moeR{l}")
            Rf = V(R, R.t[:].bitcast(F32)) if False else None
            off = 0
            hblk, off = carve(R, off, [128, KC, TB], 2, "hblk")
            wg = []
            for i in range(4):
                b_, off = carve(R, off, [128, KC * 128], 2, f"wg{i}")
                wg.append(b_)
            gu_bufs = [hblk] + wg
            off2 = 0
            wd = []
            for i in range(3):
                b_, off2 = carve(R, off2, [128, 48 * 128], 2, f"wd{i}")
                wd.append(b_)
            dn_bufs = list(wd)
            for a_ in gu_bufs:
                a_.aliases = list(dn_bufs)
            for a_ in dn_bufs:
                a_.aliases = list(gu_bufs)
            actT = p.sb(st, [128, 96, TB], BF16)
            t = {k: p.sb(st, [128, TB], F32, name=f"moe{l}_{k}") for k in ["glu", "sig", "lin", "a1"]}
            hr = [p.sb(st, [128, TB], F32) for _ in range(2)]
            zt = [t["glu"], t["sig"]]
            zq = [t["lin"], t["a1"]]
            self.h16t = [p.sb(st, [128, TB], BF16) for _ in range(2)]
            stat = [p.sb(st, [128, TB], F32) for _ in range(4)]
            lnp = p.sb(st, [128, 2, KC], F32)
            bgu = p.sb(st, [128, NE, 6], F32)
            wr = p.sb(st, [128, KC, NE], F32)
            br = p.sb(st, [NE, 1], F32)
            lgT = p.sb(st, [NE, TB], F32)
            lg = p.sb(st, [128, 4, NE], F32)
            gt = p.sb(st, [128, 4, NE], F32)
            m8 = p.sb(st, [128, 8], F32)
            sc = p.sb(st, [128, 4], F32)
            gT32 = p.sb(st, [NE, TB], F32)
            bdc = [p.sb(st, [NE, 128], F32) for _ in range(2)]
            p.dma(lnp[:], ln_in[:])
            p.dma(bgu[:], b_gu[:])
            p.dma(wr[:], w_r[:])
            p.dma(br[:], b_r[:])
            wi = 0
            for b in range(NBLK):
                sl = slice(b * TB, (b + 1) * TB)
                p.dma(hblk[:], self.H16[1][:, :, sl].re("c p t -> p c t"))
                for k in range(KC):
                    h = hr[k % 2]
                    p.dma(h[:], self.H32[1][k, :, sl])
                    p.mm(ps[6][0:NE, :], wr[:, k, :], h[:], start=(k == 0), stop=(k == KC - 1))
                p.act(lgT[:], ps[6][0:NE, :], AF.Identity, bias=br[:, 0:1])
                for j in range(4):
                    p.tr(ps[5][:, j * NE:(j + 1) * NE], lgT[:, j * 128:(j + 1) * 128], self.ident32[0:NE, 0:NE])
                p.copy(lg[:].re("p j e -> p (j e)"), ps[5][:, 0:4 * NE], eng="act")
                for j in range(4):
                    p.op("dve", lambda E: E.max(out=m8[:].ap, in_=lg[:, j, :].ap), reads=[lg], writes=[m8])
                    p.ts(gt[:, j, :], lg[:, j, :], m8[:, 3:4], ALU.is_ge)
                    p.ts(sc[:, 0:1], m8[:, 0:1], -1.0, ALU.mult)
                    p.act(lg[:, j, :], lg[:, j, :], AF.Exp, bias=sc[:, 0:1])
                    p.tt(gt[:, j, :], gt[:, j, :], lg[:, j, :], ALU.mult)
                    p.op("dve", lambda E: E.tensor_reduce(out=sc[:, 1:2].ap, in_=gt[:, j, :].ap, axis=AX.X, op=ALU.add),
                         reads=[gt], writes=[sc])
                    p.recip(sc[:, 2:3], sc[:, 1:2])
                    p.ts(gt[:, j, :], gt[:, j, :], sc[:, 2:3], ALU.mult)
                    p.tr(ps[5][0:NE, j * 128:(j + 1) * 128], gt[:, j, :], self.ident32[:])
                p.copy(gT32[:], ps[5][0:NE, :], eng="act")
                for e in range(NE):
                    p.mm(ps[4][:], self.ident32[0:NE, e:e + 1].bc([NE, 128]), gT32[:])
                    for cc in range(3):
                        wgs = wg[wi % 4]
                        wls = wg[(wi + 1) % 4]
                        wi += 2
                        p.dma(wgs[:], wgu16[e * 6 + cc, :, :])
                        p.dma(wls[:], wgu16[e * 6 + 3 + cc, :, :])
                        pg = ps[(cc % 2) * 2]
                        pl = ps[(cc % 2) * 2 + 1]
                        for k in range(KC):
                            p.mm(pg[:], wgs[:, k * 128:(k + 1) * 128], hblk[:, k, :], start=(k == 0), stop=(k == KC - 1))
                        for k in range(KC):
                            p.mm(pl[:], wls[:, k * 128:(k + 1) * 128], hblk[:, k, :], start=(k == 0), stop=(k == KC - 1))
                        p.ts(t["glu"][:], pg[:], bgu[:, e, cc:cc + 1], ALU.add, LIM, ALU.min)
                        p.act(t["sig"][:], t["glu"][:], AF.Sigmoid, scale=SW_ALPHA)
                        p.ts(t["lin"][:], pl[:], bgu[:, e, 3 + cc:4 + cc], ALU.add, LIM, ALU.min)
                        p.ts(t["lin"][:], t["lin"][:], -LIM, ALU.max, 1.0, ALU.add)
                        p.tt(t["a1"][:], t["glu"][:], t["sig"][:], ALU.mult)
                        p.tt(t["a1"][:], t["a1"][:], t["lin"][:], ALU.mult, eng="pool")
                        p.tt(actT[:, e * 3 + cc, :], t["a1"][:], ps[4][:], ALU.mult)

                def extra(n, pst):
                    bb = bdc[n % 2]
                    p.dma(bb[:], b_dn[:, n * 128:(n + 1) * 128])
                    p.mm(pst[:], bb[:], gT32[:], start=False, stop=True)
                self.proj_res_ln(st, b, actT, 96, wdn16, 2, extra, self.H32[1], 0, lnp[:, 0, :], lnp[:, 1, :],
                                 wd, hr, zt, zq, [ps[0], ps[1]], ps[2], ps[3], stat)
        p.barrier()


def host_consts(cfg):
    tri64 = (np.arange(64)[:, None] <= np.arange(64)[None, :]).astype(np.float32)
    rmask = np.ones((128, TB), np.float32)
    rmask[:, ::HG_C] = 0.0
    sbm = np.zeros((4, 128, TB), np.float32)
    for r in range(4):
        sbm[r] = ((r * 128 + np.arange(128))[:, None] < np.arange(TB)[None, :])
    triincl = -(np.arange(128)[:, None] >= np.arange(128)[None, :]).astype(np.float32)
    return {"c_ident32": np.eye(128, dtype=np.float32), "c_tri64": tri64, "c_rmask": rmask,
            "c_sbmask": sbm, "c_triincl": triincl}


def host_inputs(cfg, inp, core, kinds):
    D, T, NB, KC = cfg.D, cfg.T, cfg.NB, cfg.KC
    m = dict(host_consts(cfg))
    x = np.asarray(inp["x"])[core * NB:(core + 1) * NB].reshape(NB * T, D)
    m["xT"] = np.ascontiguousarray(x.T).reshape(KC, 128, NB * T)
    lbr = np.asarray(inp["hg_lb_raw"])
    m["lb_raw"] = np.ascontiguousarray(lbr.reshape(cfg.DEPTH, 16, 128).transpose(2, 0, 1))
    for l in range(cfg.DEPTH):
        if kinds[l] == "even":
            e = l // 2
            w = np.asarray(inp["ev_w_in"][e])
            cols = lambda a, b: w[:, a * 2048:b * 2048]
            fm = np.concatenate([cols(0, 1), cols(1, 2), cols(3, 4), cols(4, 5), cols(6, 7)], axis=1)
            tm = np.concatenate([cols(2, 3), cols(5, 6)], axis=1)
            m[f"ev_win_fm{l}"] = fm_layout(fm).reshape(80, 128, KC * 128)
            m[f"ev_win_tm{l}"] = tm_layout(tm, 256).reshape(16, 128, KC * 256)
            m[f"ev_wout{l}"] = fm_layout(np.asarray(inp["ev_w_out"][e])).reshape(KC, 128, 32 * 128)
            m[f"hg_ng{l}"] = np.asarray(inp["hg_norm_g"][e]).reshape(128, 1)
        m[f"ln1_{l}"] = np.ascontiguousarray(np.stack([pvec(np.asarray(inp["ln1_g"][l])), pvec(np.asarray(inp["ln1_b"][l]))], 1))
        m[f"ln2_{l}"] = np.ascontiguousarray(np.stack([pvec(np.asarray(inp["ln2_g"][l])), pvec(np.asarray(inp["ln2_b"][l]))], 1))
        wgu = np.asarray(inp["exp_w_gu"][l])
        wgu = wgu.reshape(NE, KC, 128, EFF, 2).transpose(0, 4, 3, 1, 2)
        wgu = wgu.reshape(NE, 2, 3, 128, KC, 128)
        wgu = wgu.transpose(0, 1, 2, 5, 4, 3)
        m[f"moe_wgu{l}"] = np.ascontiguousarray(wgu).reshape(NE * 6, 128, KC * 128)
        bgu = np.asarray(inp["exp_b_gu"][l]).reshape(NE, EFF, 2).transpose(0, 2, 1).reshape(NE, 6, 128)
        m[f"moe_bgu{l}"] = np.ascontiguousarray(bgu.transpose(2, 0, 1))
        wd = np.asarray(inp["exp_w_down"][l]).reshape(NE, 3, 128, KC, 128)
        m[f"moe_wdn{l}"] = np.ascontiguousarray(wd.transpose(3, 2, 0, 1, 4)).reshape(KC, 128, 96 * 128)
        m[f"moe_bdn{l}"] = np.ascontiguousarray(np.asarray(inp["exp_b_down"][l]))
        m[f"moe_wr{l}"] = np.ascontiguousarray(np.asarray(inp["router_w"][l]).reshape(KC, 128, NE).transpose(1, 0, 2))
        m[f"moe_br{l}"] = np.asarray(inp["router_b"][l]).reshape(NE, 1)
    return m


_CACHE = {}


def run_model(cfg, inp, kinds=None):
    kinds = kinds or ["even" if l % 2 == 0 else "odd" for l in range(cfg.DEPTH)]
    key = (cfg.D, cfg.T, cfg.NB, cfg.NCORE, cfg.DEPTH, tuple(kinds))
    if key not in _CACHE:
        _CACHE[key] = Builder(cfg, kinds).build()
    bld = _CACHE[key]
    shared = None
    in_maps = []
    for core in range(cfg.NCORE):
        if shared is None:
            m = host_inputs(cfg, inp, core, kinds)
            m = {k: np.ascontiguousarray(v, dtype=np.float32).reshape(bld.shapes[k]) for k, v in m.items() if k in bld.shapes}
            assert set(m) == set(bld.shapes), (set(bld.shapes) - set(m), set(m) - set(bld.shapes))
            shared = m
        else:
            m = dict(shared)
            x = np.asarray(inp["x"])[core * cfg.NB:(core + 1) * cfg.NB].reshape(cfg.NTOK, cfg.D)
            m["xT"] = np.ascontiguousarray(x.T, dtype=np.float32).reshape(bld.shapes["xT"])
        in_maps.append(m)
    res = run_bass_kernel_spmd(bld.p.nc, in_maps, core_ids=list(range(cfg.NCORE)))
    outs = []
    for core in range(cfg.NCORE):
        oT = np.asarray(res.results[core]["outT"]).reshape(cfg.D, cfg.NTOK)
        outs.append(np.ascontiguousarray(oT.T).reshape(cfg.NB, cfg.T, cfg.D))
    return np.concatenate(outs, 0)


NCORE_USED = 4


def kernel(**inputs):
    B, T, D = inputs["x"].shape
    ncore = NCORE_USED
    cfg = Cfg(D=D, T=T, NB=B // ncore, NCORE=ncore, DEPTH=inputs["ln1_g"].shape[0])
    return run_model(cfg, inputs).astype(np.float32)


CMP_LEN, CMP_STRIDE, SLC, NSEL, WIN = 32, 16, 64, 16, 512
OFF_S = 511
LS = 1536
OFF_C = 2063
LC = 4608
NEG = -1e30
FORCED = 1e9


def _bucket(dist):
    dist = np.maximum(dist, 0)
    r = np.log(np.maximum(dist, 16).astype(np.float32) / np.float32(16)) / np.float32(math.log(128 / 16))
    large = np.minimum(16 + (r * np.float32(16)).astype(np.int32), 31)
    return np.where(dist < 16, dist, large)


def nsa_consts(cfg):
    T = cfg.T
    m = {}
    d = np.arange(LS) - OFF_S
    oh = np.zeros((32, LS), np.float32)
    ok = d >= 0
    oh[_bucket(d)[ok], np.arange(LS)[ok]] = 1.0
    m["c_ohS"] = oh
    ohw = oh.copy()
    ohw[:, d >= WIN] = 0.0
    m["c_ohW"] = ohw
    d = np.arange(LC) - OFF_C
    oh = np.zeros((32, LC), np.float32)
    ok = d >= 0
    oh[_bucket(d)[ok], np.arange(LC)[ok]] = 1.0
    m["c_ohC"] = oh
    m["c_antiI"] = np.eye(128, dtype=np.float32)[::-1].copy()
    ncmp = (T - CMP_LEN) // CMP_STRIDE + 1
    nslc = T // SLC
    cs = np.arange(ncmp) * CMP_STRIDE
    ce = cs + CMP_LEN - 1
    ss = np.arange(nslc) * SLC
    ov = ((cs[:, None] < ss[None, :] + SLC) & (ce[:, None] >= ss[None, :])).astype(np.float32)
    nch = (ncmp + 127) // 128
    ovp = np.zeros((nch * 128, nslc), np.float32)
    ovp[:ncmp] = ov
    m["c_overlap"] = np.ascontiguousarray(ovp.reshape(nch, 128, nslc).transpose(1, 0, 2))
    NT = T // TB
    sm = np.zeros((NT, 3, 128, 4, nslc), np.float32)
    for i in range(NT):
        t = i * TB + (np.arange(4)[None, :] * 128 + np.arange(128)[:, None])
        qb = t // SLC
        j = np.arange(nslc)[None, None, :]
        valid = j <= qb[:, :, None]
        forced = (j == 0) | (j == qb[:, :, None]) | (j == qb[:, :, None] - 1)
        sm[i, 0] = (valid & ~forced)
        sm[i, 1] = np.where(valid & forced, FORCED, np.where(valid, 0.0, NEG))
        sm[i, 2] = valid
    m["c_selmask"] = sm
    m["c_expand"] = (np.arange(nslc)[:, None] == (np.arange(T)[None, :] // SLC)).astype(np.float32)
    return m


def _odd_layer(self, l):
    c, p = self.c, self.p
    D, T, NB, KC, NTOK, NBLK = c.D, c.T, c.NB, c.KC, c.NTOK, c.NBLK
    BPS = T // TB
    NT = T // TB
    NCMP = (T - CMP_LEN) // CMP_STRIDE + 1
    NCC = (NCMP + 127) // 128
    NSLC = T // SLC
    ps = self.ps
    w_fm = self.inp(f"od_win_fm{l}", [49, 128, KC * 128])
    w_tm = self.inp(f"od_win_tm{l}", [4, 128, KC * 256])
    w_out = self.inp(f"od_wout{l}", [KC, 128, 32 * 128])
    ln_in = self.inp(f"ln1_{l}", [128, 2, KC])
    cw1 = [self.inp(f"cmp_w1_{l}_{i}", [128, 32, 128]) for i in range(2)]
    cw2 = [self.inp(f"cmp_w2_{l}_{i}", [128, 128]) for i in range(2)]
    cpos = [self.inp(f"cmp_pos_{l}_{i}", [128, 32]) for i in range(2)]
    qT = self.scratch(f"qT{l}", [NB, 32, 128, T], BF16)
    kcT = self.scratch(f"kcT{l}", [NB, 4, 128, T], BF16)
    vcT = self.scratch(f"vcT{l}", [NB, 4, 128, T], BF16)
    ksT = self.scratch(f"ksT{l}", [NB, 4, 128, T], BF16)
    kwT = self.scratch(f"kwT{l}", [NB, 4, 128, T], BF16)
    gTd = self.scratch(f"gTd{l}", [NB, 128, T], F32)
    vsTM = self.scratch(f"vsTM{l}", [NB, T, 512], BF16)
    vwTM = self.scratch(f"vwTM{l}", [NB, T, 512], BF16)
    ocT = self.scratch(f"ocT{l}", [8, 128, T], F32)
    with ExitStack() as st:
        self.cf32 = [p.sb(st, [128, 4096], F32) for _ in range(2)]
        self.cb16 = [p.sb(st, [128, 4096], BF16) for _ in range(2)]
        wfm16 = self.cast_w(st, w_fm, [49, 128, KC * 128])
        wtm16 = self.cast_w(st, w_tm, [4, 128, KC * 256])
        wout16 = self.cast_w(st, w_out, [KC, 128, 32 * 128])
    p.barrier()
    if not hasattr(self, "nsa_M"):
        self.nsa_prep()
    with ExitStack() as st:
        hb = [p.sb(st, [128, KC, TB], BF16) for _ in range(2)]
        ws = [p.sb(st, [128, KC * 128], BF16) for _ in range(3)]
        wt = [p.sb(st, [128, KC * 256], BF16) for _ in range(2)]
        o16 = [p.sb(st, [128, TB], BF16) for _ in range(4)]
        o32 = [p.sb(st, [128, TB], F32) for _ in range(2)]
        wi = 0
        oi = 0
        for b in range(NBLK):
            s, tb = divmod(b, BPS)
            h = hb[b % 2]
            tsl = slice(tb * TB, (tb + 1) * TB)
            p.dma(h[:], self.H16[0][:, :, b * TB:(b + 1) * TB].re("c p t -> p c t"))
            for mc in range(49):
                w = ws[wi % 3]
                wi += 1
                pst = ps[mc % 4]
                p.dma(w[:], wfm16[mc, :, :])
                for k in range(KC):
                    p.mm(pst[:], w[:, k * 128:(k + 1) * 128], h[:, k, :], start=(k == 0), stop=(k == KC - 1))
                if mc == 48:
                    o = o32[b % 2]
                    p.act(o[:], pst[:], AF.Sigmoid)
                    p.dma(gTd[s, :, tsl], o[:])
                    continue
                o = o16[oi % 4]
                oi += 1
                if mc < 32:
                    p.act(o[:], pst[:], AF.Copy, scale=HD ** -0.5)
                    p.dma(qT[s, mc, :, tsl], o[:])
                else:
                    p.copy(o[:], pst[:], eng="dve" if mc % 2 else "act")
                    dst = [kcT, vcT, ksT, kwT][(mc - 32) // 4]
                    p.dma(dst[s, (mc - 32) % 4, :, tsl], o[:])
            for sb_ in range(4):
                w = wt[sb_ % 2]
                p.dma(w[:], wtm16[sb_, :, :])
                dst = vsTM if sb_ < 2 else vwTM
                col = (sb_ % 2) * 256
                for j2 in range(2):
                    pst = ps[4 + (j2 % 2)]
                    for jj in range(2):
                        j = j2 * 2 + jj
                        for k in range(KC):
                            p.mm(pst[:, jj * 256:(jj + 1) * 256], h[:, k, j * 128:(j + 1) * 128],
                                 w[:, k * 256:(k + 1) * 256], start=(k == 0), stop=(k == KC - 1))
                    o = o16[oi % 4]
                    oi += 1
                    p.copy(o[:], pst[:], eng="act" if j2 else "dve")
                    r0 = tb * TB + j2 * 256
                    p.dma(dst[s, r0:r0 + 256, col:col + 256].re("(j p) n -> p j n", p=128), o[:].re("p (j n) -> p j n", j=2))
    p.barrier()
    Ms, Mw, Mc, b31 = self.nsa_M
    with ExitStack() as st:
        w1 = [p.sb(st, [128, 32, 128], BF16) for _ in range(2)]
        w2 = [p.sb(st, [128, 128], BF16) for _ in range(2)]
        posT = [p.sb(st, [128, 32], BF16) for _ in range(2)]
        cvec = [p.sb(st, [128, 1], F32) for _ in range(2)]
        tmpf = p.sb(st, [128, 32, 128], F32)
        ones16 = p.sb(st, [128, 128], BF16)
        p.memset(ones16[:], 1.0)
        for i in range(2):
            p.dma(tmpf[:], cw1[i][:])
            p.copy(w1[i][:], tmpf[:])
            p.dma(tmpf[:, 0, :], cw2[i][:])
            p.copy(w2[i][:], tmpf[:, 0, :])
            p.dma(tmpf[:, 1, 0:32], cpos[i][:])
            p.copy(posT[i][:], tmpf[:, 1, 0:32])
            for ll in range(32):
                p.mm(ps[6][:, 0:1], w1[i][:, ll, :], posT[i][:, ll:ll + 1], start=(ll == 0), stop=(ll == 31))
            p.copy(cvec[i][:], ps[6][:, 0:1], eng="act")
        ovl = p.sb(st, [128, NCC, NSLC], BF16)
        ovf = p.sb(st, [128, NCC, NSLC], F32)
        p.dma(ovf[:], self.c_overlap[:])
        p.copy(ovl[:], ovf[:])
        expd = p.sb(st, [NSLC, T], BF16)
        for q0 in range(0, T, 2048):
            q1 = min(T, q0 + 2048)
            p.dma(tmpf[0:NSLC, :, :].re("p a b -> p (a b)")[:, 0:q1 - q0], self.c_expand[:, q0:q1])
            p.copy(expd[:, q0:q1], tmpf[0:NSLC, :, :].re("p a b -> p (a b)")[:, 0:q1 - q0])
        selm = p.sb(st, [128, 3, 4, NSLC], F32)
        xin = p.sb(st, [128, T], BF16)
        hid = p.sb(st, [128, NCC * 128], BF16)
        kcmp = p.sb(st, [128, NCC * 128], BF16)
        vcmp = p.sb(st, [128, NCC, 128], BF16)
        kst = p.sb(st, [128, T], BF16)
        kwt = p.sb(st, [128, T], BF16)
        vst = p.sb(st, [128, T // 128, 128], BF16)
        vwt = p.sb(st, [128, T // 128, 128], BF16)
        qg = [p.sb(st, [128, 8, TB], BF16) for _ in range(2)]
        q1h = [p.sb(st, [128, TB], BF16) for _ in range(2)]
        gt32 = [p.sb(st, [128, TB], F32) for _ in range(2)]
        selT = p.sb(st, [NSLC, T], BF16)
        ee = [p.sb(st, [128, TB], BF16) for _ in range(3)]
        pT = [p.sb(st, [128, TB], BF16) for _ in range(4)]
        mt = [p.sb(st, [128, TB], BF16) for _ in range(4)]
        msel = [p.sb(st, [128, 5, TB], BF16) for _ in range(1)]
        mwin = [p.sb(st, [128, 8, TB], BF16) for _ in range(1)]
        rz = p.sb(st, [128, TB], F32)
        fac = p.sb(st, [128, TB], F32)
        acc = p.sb(st, [128, TB], F32)
        tmp = p.sb(st, [128, TB], F32)
        o16 = [p.sb(st, [128, TB], BF16) for _ in range(2)]
        impT = p.sb(st, [NSLC, TB], F32)
        imp = p.sb(st, [128, 4, NSLC], F32)
        imp2 = p.sb(st, [128, 4, NSLC], F32)
        selt = p.sb(st, [128, 4, NSLC], F32)
        m8 = p.sb(st, [128, 8], F32)
        ei = 0
        pi = 0
        mi = 0

        def nxt(lst, idx):
            return lst[idx % len(lst)]
        for s in range(NB):
            for g in range(4):
                for i, src in enumerate([kcT, vcT]):
                    p.dma(xin[:], src[s, g, :, :])
                    for ll in range(32):
                        p.mm(ps[5][:, 0:NCMP], w1[i][:, ll, :], xin[:, ll:ll + CMP_STRIDE * (NCMP - 1) + 1:CMP_STRIDE],
                             start=(ll == 0), stop=(ll == 31))
                    p.act(hid[:, 0:NCMP], ps[5][:, 0:NCMP], AF.Silu, bias=cvec[i][:, 0:1])
                    if i == 0:
                        p.mm(ps[5][:, 0:NCMP], w2[i][:], hid[:, 0:NCMP])
                        p.copy(kcmp[:, 0:NCMP], ps[5][:, 0:NCMP], eng="act")
                    else:
                        for ch in range(NCC):
                            nn = min(128, NCMP - ch * 128)
                            p.mm(ps[5][0:nn, 0:128], hid[:, ch * 128:ch * 128 + nn], w2[i][:])
                            p.copy(vcmp[0:nn, ch, :], ps[5][0:nn, 0:128], eng="act")
                p.dma(kst[:], ksT[s, g, :, :])
                p.dma(kwt[:], kwT[s, g, :, :])
                p.dma(vst[:], vsTM[s, :, g * 128:(g + 1) * 128].re("(c p) d -> p c d", p=128))
                p.dma(vwt[:], vwTM[s, :, g * 128:(g + 1) * 128].re("(c p) d -> p c d", p=128))
                for i in range(NT):
                    tsl = slice(i * TB, (i + 1) * TB)
                    qq = qg[i % 2]
                    gg = gt32[i % 2]
                    p.dma(qq[:], qT[s, g * 8:(g + 1) * 8, :, tsl].re("h p t -> p h t"))
                    p.dma(gg[:], gTd[s, :, tsl])
                    p.dma(selm[:], self.c_selmask[i].re("a p j m -> p a j m"))
                    nmax = (i * TB + TB - 1 - (CMP_LEN - 1)) // CMP_STRIDE
                    chs = [ch for ch in range(NCC) if ch * 128 <= min(nmax, NCMP - 1)]
                    for r in range(8):
                        hh = g * 8 + r
                        pts = []
                        for ci, ch in enumerate(chs):
                            nn = min(128, NCMP - ch * 128)
                            delta = i - 4 * ch
                            pa = ps[pi % 2]
                            pi += 1
                            p.mm(pa[0:nn, :], kcmp[:, ch * 128:ch * 128 + nn], qq[:, r, :])
                            pt = nxt(pT, ei)
                            ei += 1
                            if delta <= 4:
                                e = nxt(ee, ei)
                                m_ = nxt(mt, mi)
                                mi += 1
                                p.dma(m_[:], Mc[delta][hh, :, :])
                                p.act(e[0:nn, :], pa[0:nn, :], AF.Exp)
                                p.tt(pt[0:nn, :], e[0:nn, :], m_[0:nn, :], ALU.mult, eng="pool")
                            else:
                                p.act(pt[0:nn, :], pa[0:nn, :], AF.Exp, bias=b31[0:nn, hh:hh + 1])
                            p.mm(ps[2][:], vcmp[0:nn, ch, :], pt[0:nn, :], start=(ci == 0), stop=(ci == len(chs) - 1))
                            p.mm(ps[3][:], ones16[0:nn, :], pt[0:nn, :], start=(ci == 0), stop=(ci == len(chs) - 1))
                            pts.append((pt, nn, ch))
                        p.ts(rz[:], ps[3][:], 1e-30, ALU.max)
                        p.recip(rz[:], rz[:])
                        p.mm(ps[5][:], self.ident32[0:96, g * 24 + r * 3:g * 24 + r * 3 + 1].bc([96, 128]), gg[0:96, :])
                        p.tt(fac[:], rz[:], ps[5][:], ALU.mult)
                        p.tt(acc[:], ps[2][:], fac[:], ALU.mult)
                        p.dma(ocT[r, :, tsl], acc[:])
                        for ci, (pt, nn, ch) in enumerate(pts):
                            p.tt(pt[0:nn, :], pt[0:nn, :], rz[0:nn, :], ALU.mult, eng="pool")
                            p.mm(ps[4][0:NSLC, :], ovl[0:nn, ch, :], pt[0:nn, :],
                                 start=(r == 0 and ci == 0), stop=(r == 7 and ci == len(pts) - 1))
                    p.copy(impT[:], ps[4][0:NSLC, :], eng="act")
                    for j in range(4):
                        p.tr(ps[6][:, j * NSLC:(j + 1) * NSLC], impT[:, j * 128:(j + 1) * 128], self.ident32[0:NSLC, 0:NSLC])
                    p.tt(imp[:].re("p j m -> p (j m)"), ps[6][:, 0:4 * NSLC], selm[:, 0, :, :].re("p j m -> p (j m)"), ALU.mult)
                    p.tt(imp[:], imp[:], selm[:, 1, :, :], ALU.add)
                    for j in range(4):
                        p.op("dve", lambda E: E.max(out=m8[:].ap, in_=imp[:, j, :].ap), reads=[imp], writes=[m8])
                        p.op("dve", lambda E: E.match_replace(out=imp2[:, j, :].ap, in_to_replace=m8[:].ap, in_values=imp[:, j, :].ap,
                                                              imm_value=-3e38), reads=[m8, imp], writes=[imp2])
                        p.op("dve", lambda E: E.max(out=m8[:].ap, in_=imp2[:, j, :].ap), reads=[imp2], writes=[m8])
                        p.ts(selt[:, j, :], imp[:, j, :], m8[:, 7:8], ALU.is_ge)
                    p.tt(selt[:], selt[:], selm[:, 2, :, :], ALU.mult)
                    for j in range(4):
                        p.tr(ps[6][0:NSLC, j * 128:(j + 1) * 128], selt[:, j, :], self.ident32[:])
                    p.copy(selT[:, tsl], ps[6][0:NSLC, :], eng="act")
                for r in range(8):
                    hh = g * 8 + r
                    p.dma(msel[0][:], Ms[:, hh, :, :].re("a p t -> p a t"))
                    p.dma(mwin[0][:], Mw[:, hh, :, :].re("a p t -> p a t"))
                    for i in range(NT):
                        tsl = slice(i * TB, (i + 1) * TB)
                        q1 = q1h[i % 2]
                        gg = gt32[i % 2]
                        p.dma(q1[:], qT[s, hh, :, tsl])
                        p.dma(gg[:], gTd[s, :, tsl])
                        p.dma(acc[:], ocT[r, :, tsl])
                        ncc = 4 * i + 4
                        for cc in range(ncc):
                            rel = 4 * i - cc
                            pa = ps[pi % 2]
                            pi += 1
                            p.mm(pa[:], kst[:, cc * 128:(cc + 1) * 128], q1[:])
                            p.mm(ps[5][:], expd[:, cc * 128:(cc + 1) * 128], selT[:, tsl])
                            e = nxt(ee, ei)
                            pt = nxt(pT, ei)
                            ei += 1
                            if rel >= 2:
                                p.act(e[:], pa[:], AF.Exp, bias=b31[:, hh:hh + 1])
                            else:
                                p.act(e[:], pa[:], AF.Exp)
                                p.tt(e[:], e[:], msel[0][:, rel + 3, :], ALU.mult, eng="pool")
                            p.tt(pt[:], e[:], ps[5][:], ALU.mult)
                            p.mm(ps[2][:], vst[:, cc, :], pt[:], start=(cc == 0), stop=(cc == ncc - 1))
                            p.mm(ps[3][:], ones16[:], pt[:], start=(cc == 0), stop=(cc == ncc - 1))
                        p.recip(rz[:], ps[3][:])
                        p.mm(ps[6][:], self.ident32[0:96, g * 24 + r * 3 + 1:g * 24 + r * 3 + 2].bc([96, 128]), gg[0:96, :])
                        p.tt(fac[:], rz[:], ps[6][:], ALU.mult)
                        p.tt(tmp[:], ps[2][:], fac[:], ALU.mult)
                        p.tt(acc[:], acc[:], tmp[:], ALU.add)
                        c0 = max(0, 4 * i - 4)
                        for cc in range(c0, ncc):
                            rel = 4 * i - cc
                            pa = ps[pi % 2]
                            pi += 1
                            p.mm(pa[:], kwt[:, cc * 128:(cc + 1) * 128], q1[:])
                            e = nxt(ee, ei)
                            pt = nxt(pT, ei)
                            ei += 1
                            p.act(e[:], pa[:], AF.Exp)
                            p.tt(pt[:], e[:], mwin[0][:, rel + 3, :], ALU.mult, eng="pool" if cc % 2 else "dve")
                            p.mm(ps[2][:], vwt[:, cc, :], pt[:], start=(cc == c0), stop=(cc == ncc - 1))
                            p.mm(ps[3][:], ones16[:], pt[:], start=(cc == c0), stop=(cc == ncc - 1))
                        p.recip(rz[:], ps[3][:])
                        p.mm(ps[6][:], self.ident32[0:96, g * 24 + r * 3 + 2:g * 24 + r * 3 + 3].bc([96, 128]), gg[0:96, :])
                        p.tt(fac[:], rz[:], ps[6][:], ALU.mult)
                        p.tt(tmp[:], ps[2][:], fac[:], ALU.mult)
                        o = o16[i % 2]
                        p.tt(o[:], acc[:], tmp[:], ALU.add)
                        p.dma(self.mixedT[hh, :, s * T + i * TB:s * T + (i + 1) * TB], o[:])
    p.barrier()
    self.out_proj_ln(l, wout16, ln_in)


def _nsa_prep(self):
    c, p = self.c, self.p
    ps = self.ps
    g = self.glob
    rb = self.inp("rel_bias", [32, 32])
    ohS = self.inp("c_ohS", [32, LS])
    ohW = self.inp("c_ohW", [32, LS])
    ohC = self.inp("c_ohC", [32, LC])
    antiI = self.inp("c_antiI", [128, 128])
    self.c_overlap = self.inp("c_overlap", [128, (((c.T - CMP_LEN) // CMP_STRIDE + 1) + 127) // 128, c.T // SLC])
    self.c_selmask = self.inp("c_selmask", [c.T // TB, 3, 128, 4, c.T // SLC])
    self.c_expand = self.inp("c_expand", [c.T // SLC, c.T])
    vS = self.scratch("vecS", [32, LS], F32)
    vW = self.scratch("vecW", [32, LS], F32)
    vC = self.scratch("vecC", [32, LC], F32)
    Ms = self.scratch("M_s", [5, 32, 128, TB], BF16)
    Mw = self.scratch("M_w", [8, 32, 128, TB], BF16)
    Mc = self.scratch("M_c", [5, 32, 128, TB], BF16)
    b31 = p.sb(g, [128, 32], F32, name="b31")
    with ExitStack() as st:
        tab = p.sb(st, [32, 32], F32)
        eb = p.sb(st, [32, 32], F32)
        oh = [p.sb(st, [32, TB], F32) for _ in range(2)]
        vo = [p.sb(st, [32, TB], F32) for _ in range(2)]
        J = p.sb(st, [128, 128], F32)
        tl = [p.sb(st, [128, TB], F32) for _ in range(2)]
        to = [p.sb(st, [128, TB], BF16) for _ in range(2)]
        p.dma(tab[:], rb[:])
        p.dma(J[:], antiI[:])
        p.act(eb[:], tab[:], AF.Exp)
        p.mm(ps[0][:, 0:32], self.ident32[0:32, 31:32].bc([32, 128]), tab[:])
        p.copy(b31[:], ps[0][:, 0:32], eng="act")
        k = 0
        for (src, dst, L) in [(ohS, vS, LS), (ohW, vW, LS), (ohC, vC, LC)]:
            for c0 in range(0, L, TB):
                a, b_ = oh[k % 2], vo[k % 2]
                p.dma(a[:], src[:, c0:c0 + TB])
                p.mm(ps[k % 2][0:32, :], eb[:], a[:])
                p.copy(b_[:], ps[k % 2][0:32, :], eng="act" if k % 2 else "dve")
                p.dma(dst[:, c0:c0 + TB], b_[:])
                k += 1
        geos = [(Ms, vS, LS, OFF_S, [(a, 128 * (a - 3) - 127, 1) for a in range(5)]),
                (Mw, vW, LS, OFF_S, [(a, 128 * (a - 3) - 127, 1) for a in range(8)]),
                (Mc, vC, LC, OFF_C, [(a, 512 * a - (16 * 127 + 31), 16) for a in range(5)])]
        k = 0
        for (M, vec, L, OFF, lst) in geos:
            for (a, base, pstep) in lst:
                for hh in range(32):
                    off = hh * L + OFF + base
                    assert off - hh * L >= 0 and off - hh * L + pstep * 127 + TB - 1 < L, (a, base, L)
                    t_, o_ = tl[k % 2], to[k % 2]
                    p.dma(t_[:], V(vec, bass.AP(vec.t, off, [[pstep, 128], [1, TB]])))
                    p.mm(ps[2 + k % 2][:], J[:], t_[:])
                    p.copy(o_[:], ps[2 + k % 2][:], eng="act" if k % 2 else "dve")
                    p.dma(M[a, hh, :, :], o_[:])
                    k += 1
    p.barrier()
    self.nsa_M = (Ms, Mw, Mc, b31)


Builder.odd_layer = _odd_layer
Builder.nsa_prep = _nsa_prep
_host_inputs_base = host_inputs


def host_inputs(cfg, inp, core, kinds):
    KC = cfg.KC
    m = _host_inputs_base(cfg, inp, core, kinds)
    if "odd" in kinds:
        m.update(nsa_consts(cfg))
        m["rel_bias"] = np.asarray(inp["rel_bias"])
    for l in range(cfg.DEPTH):
        if kinds[l] != "odd":
            continue
        o = l // 2
        w = np.asarray(inp["od_w_in"][o])
        gates = np.zeros((w.shape[0], 128), np.float32)
        gates[:, :96] = w[:, 7168:7264]
        fm = np.concatenate([w[:, 0:4096], w[:, 4096:4608], w[:, 4608:5120], w[:, 5120:5632], w[:, 6144:6656], gates], axis=1)
        tm = np.concatenate([w[:, 5632:6144], w[:, 6656:7168]], axis=1)
        m[f"od_win_fm{l}"] = fm_layout(fm).reshape(49, 128, KC * 128)
        m[f"od_win_tm{l}"] = tm_layout(tm, 256).reshape(4, 128, KC * 256)
        m[f"od_wout{l}"] = fm_layout(np.asarray(inp["od_w_out"][o])).reshape(KC, 128, 32 * 128)
        for i, nm in enumerate(["k", "v"]):
            w1 = np.asarray(inp[f"cmp_{nm}_w1"][o]).reshape(32, 128, 128)
            m[f"cmp_w1_{l}_{i}"] = np.ascontiguousarray(w1.transpose(1, 0, 2))
            m[f"cmp_w2_{l}_{i}"] = np.asarray(inp[f"cmp_{nm}_w2"][o])
            m[f"cmp_pos_{l}_{i}"] = np.ascontiguousarray(np.asarray(inp[f"cmp_{nm}_pos"][o]).T)
    return m
```

```python
import math
from contextlib import ExitStack
import numpy as np
import concourse.bass as bass
import concourse.mybir as mybir
from concourse.bass_utils import run_bass_kernel_spmd

F32 = mybir.dt.float32
BF16 = mybir.dt.bfloat16
AF = mybir.ActivationFunctionType
ALU = mybir.AluOpType
AX = mybir.AxisListType


class V:
    def __init__(self, buf, ap):
        self.buf = buf
        self.ap = ap

    def __getitem__(self, k):
        return V(self.buf, self.ap[k])

    def re(self, pat, **kw):
        return V(self.buf, self.ap.rearrange(pat, **kw))

    def bc(self, shape):
        return V(self.buf, self.ap.to_broadcast(list(shape)))

    def un(self, ax):
        return V(self.buf, self.ap.unsqueeze(ax))


class Buf:
    def __init__(self, t, name):
        self.t = t
        self.name = name
        self.w = None
        self.r = {}
        self.aliases = []

    def __getitem__(self, k):
        return V(self, self.t[k])


class SubBuf(Buf):
    def __init__(self, ap, name):
        Buf.__init__(self, None, name)
        self.base = ap

    def __getitem__(self, k):
        return V(self, self.base[k])


def carve(region, off, shape, dt_size, name):
    n = int(np.prod(shape[1:]))
    ap = region.t[0:shape[0], off:off + n]
    if len(shape) == 3:
        ap = ap.rearrange("p (a b) -> p a b", a=shape[1])
    return SubBuf(ap, name), off + n


class Prog:
    ENG = ("pe", "act", "dve", "pool", "sp")
    NDSEM = 96

    def __init__(self):
        self.nc = bass.Bass("TRN2", target_bir_lowering=False)
        nc = self.nc
        self.e = {"pe": nc.tensor, "act": nc.scalar, "dve": nc.vector, "pool": nc.gpsimd, "sp": nc.sync}
        self.sem = {k: nc.alloc_semaphore(f"S_{k}") for k in self.ENG}
        self.cnt = {k: 0 for k in self.ENG}
        self.waited = {k: {} for k in self.ENG}
        self.pend = {k: {} for k in self.ENG}
        self.fold = True
        self.fold_dma = True
        self.nfold = 1
        self.fold_engs = ('dve', 'act', 'pool', 'pe')
        self.same_sync = True
        self.nsame = 0
        self.dsem = [[nc.alloc_semaphore(f"D{i}"), 0] for i in range(self.NDSEM)]
        self.dnext = 0
        self.nbuf = 0
        self.ninst = 0

    def sb(self, st, shape, dt=F32, name=None):
        self.nbuf += 1
        name = name or f"sb{self.nbuf}"
        return Buf(st.enter_context(self.nc.sbuf_tensor(name, list(shape), dt)), name)

    def ps(self, st, shape, dt=F32, name=None):
        self.nbuf += 1
        name = name or f"ps{self.nbuf}"
        return Buf(st.enter_context(self.nc.psum_tensor(name, list(shape), dt)), name)

    def dram(self, name, shape, dt=F32, kind="Internal"):
        b = Buf(self.nc.dram_tensor(name, list(shape), dt, kind=kind), name)
        b.is_dram = True
        return b

    def _wait(self, eng, tok):
        if tok is None:
            return
        kind, key, val = tok
        if kind == "eng" and key == eng and (eng == "pe" or not self.same_sync):
            return
        if kind == "eng" and key == eng:
            self.nsame += 1
        wk = (kind, key)
        if self.waited[eng].get(wk, 0) >= val:
            return
        self.waited[eng][wk] = val
        self.pend[eng][wk] = val

    def _flush(self, eng, keep_last=False):
        items = list(self.pend[eng].items())
        self.pend[eng] = {}
        held = None
        if keep_last and items:
            held = items[-self.nfold:]
            items = items[:-self.nfold]
        for (kind, key), val in items:
            sem = self.sem[key] if kind == "eng" else self.dsem[key][0]
            self.e[eng].wait_ge(sem, val)
            self.ninst += 1
            self.nwait = getattr(self, "nwait", 0) + 1
        return held

    def _deps(self, eng, reads, writes):
        for b in reads:
            self._wait(eng, b.w)
        for b in writes:
            for x in [b] + b.aliases:
                self._wait(eng, x.w)
                for (kind, key), val in x.r.items():
                    self._wait(eng, (kind, key, val))

    def _record(self, tok, reads, writes):
        for b in reads:
            b.r[(tok[0], tok[1])] = tok[2]
        for b in writes:
            b.w = tok
            b.r = {}

    def op(self, eng, fn, reads=(), writes=()):
        self._deps(eng, reads, writes)
        held = self._flush(eng, keep_last=(self.fold and eng in self.fold_engs))
        ins = fn(self.e[eng])
        for (kind, key), val in (held or []):
            ins._wait_ge(self.sem[key] if kind == "eng" else self.dsem[key][0], val)
        self.cnt[eng] += 1
        ins.then_inc(self.sem[eng], 1)
        self.ninst += 1
        self._record(("eng", eng, self.cnt[eng]), reads, writes)
        return ins

    def dma(self, out, in_, eng="sp", **kw):
        if getattr(out.buf, "is_dram", False) and not getattr(in_.buf, "is_dram", False):
            eng = "pool"
        i = self.dnext
        self.dnext = (self.dnext + 1) % self.NDSEM
        self._wait(eng, ("dma", i, self.dsem[i][1]))
        self._deps(eng, [in_.buf], [out.buf])
        held = self._flush(eng, keep_last=(self.fold_dma and eng == "sp"))
        ins = self.e[eng].dma_start(out=out.ap, in_=in_.ap, **kw)
        for (kind, key), val in (held or []):
            ins._wait_ge(self.sem[key] if kind == "eng" else self.dsem[key][0], val)
        self.dsem[i][1] += 16
        ins.then_inc(self.dsem[i][0], 16)
        self.ninst += 1
        self.ndma = getattr(self, 'ndma', 0) + 1
        self._record(("dma", i, self.dsem[i][1]), [in_.buf], [out.buf])
        return ins

    def barrier(self):
        for e in self.ENG:
            for e2 in self.ENG:
                if e2 != e:
                    self._wait(e, ("eng", e2, self.cnt[e2]))
            for i in range(self.NDSEM):
                self._wait(e, ("dma", i, self.dsem[i][1]))
            self._flush(e)

    def mm(self, out, lhsT, rhs, start=True, stop=True):
        return self.op("pe", lambda E: E.matmul(out.ap, lhsT=lhsT.ap, rhs=rhs.ap, start=start, stop=stop),
                       reads=[lhsT.buf, rhs.buf], writes=[out.buf])

    def tr(self, out, in_, ident):
        return self.op("pe", lambda E: E.transpose(out.ap, in_.ap, ident.ap), reads=[in_.buf, ident.buf], writes=[out.buf])

    def act(self, out, in_, func, scale=None, bias=None, eng="act"):
        kw = {}
        rd = [in_.buf]
        if scale is not None:
            if isinstance(scale, V):
                kw["scale"] = scale.ap
                rd.append(scale.buf)
            else:
                kw["scale"] = float(scale)
        if bias is not None:
            if isinstance(bias, V):
                kw["bias"] = bias.ap
                rd.append(bias.buf)
            else:
                kw["bias"] = float(bias)
        return self.op(eng, lambda E: E.activation(out=out.ap, in_=in_.ap, func=func, **kw), reads=rd, writes=[out.buf])

    def tt(self, out, in0, in1, op, eng="dve"):
        return self.op(eng, lambda E: E.tensor_tensor(out=out.ap, in0=in0.ap, in1=in1.ap, op=op),
                       reads=[in0.buf, in1.buf], writes=[out.buf])

    def ts(self, out, in0, s1, op0, s2=None, op1=None, eng="dve"):
        rd = [in0.buf]
        a1 = s1
        if isinstance(s1, V):
            rd.append(s1.buf)
            a1 = s1.ap
        a2 = s2
        if isinstance(s2, V):
            rd.append(s2.buf)
            a2 = s2.ap
        if op1 is None:
            return self.op(eng, lambda E: E.tensor_scalar(out=out.ap, in0=in0.ap, scalar1=a1, scalar2=None, op0=op0),
                           reads=rd, writes=[out.buf])
        return self.op(eng, lambda E: E.tensor_scalar(out=out.ap, in0=in0.ap, scalar1=a1, scalar2=a2, op0=op0, op1=op1),
                       reads=rd, writes=[out.buf])

    def stt(self, out, in0, scalar, in1, op0, op1):
        rd = [in0.buf, in1.buf]
        a = scalar
        if isinstance(scalar, V):
            rd.append(scalar.buf)
            a = scalar.ap
        return self.op("dve", lambda E: E.scalar_tensor_tensor(out=out.ap, in0=in0.ap, scalar=a, in1=in1.ap, op0=op0, op1=op1),
                       reads=rd, writes=[out.buf])

    def copy(self, out, in_, eng="dve"):
        if eng == "act":
            return self.act(out, in_, AF.Copy)
        return self.op(eng, lambda E: E.tensor_copy(out=out.ap, in_=in_.ap), reads=[in_.buf], writes=[out.buf])

    def memset(self, out, val, eng="dve"):
        return self.op(eng, lambda E: E.memset(out.ap, float(val)), writes=[out.buf])

    def recip(self, out, in_):
        return self.op("dve", lambda E: E.reciprocal(out=out.ap, in_=in_.ap), reads=[in_.buf], writes=[out.buf])


HD = 128
SB_H = 16
HG_H = 16
EVEN_IN = 14336
NSA_H = 32
NSA_G = 4
ODD_IN = 7264
NE = 32
EFF = 384
LIM = 7.0
SW_ALPHA = 1.702
LN_EPS = 1e-5
F_MIN = 1e-6
HG_C = 64
TB = 512


class Cfg:
    def __init__(self, D=4096, T=4096, NB=4, NCORE=1, DEPTH=4):
        self.D, self.T, self.NB, self.NCORE, self.DEPTH = D, T, NB, NCORE, DEPTH
        self.KC = D // 128
        self.NTOK = NB * T
        self.NBLK = self.NTOK // TB
        self.alpha = (2 * DEPTH) ** 0.25


def fm_layout(w):
    K, M = w.shape
    return np.ascontiguousarray(w.reshape(K // 128, 128, M // 128, 128).transpose(2, 1, 0, 3))


def tm_layout(w, nw=256):
    K, N = w.shape
    return np.ascontiguousarray(w.reshape(K // 128, 128, N // nw, nw).transpose(2, 1, 0, 3))


def pvec(v):
    return np.ascontiguousarray(v.reshape(-1, 128).T)


class Builder:
    def __init__(self, cfg, layer_kinds=None):
        self.c = cfg
        self.p = Prog()
        self.shapes = {}
        self.kinds = layer_kinds or ["even" if l % 2 == 0 else "odd" for l in range(cfg.DEPTH)]
        self.ncast = 0

    def inp(self, name, shape, dt=F32):
        b = self.p.dram(name, shape, dt, kind="ExternalInput")
        self.shapes[name] = tuple(shape)
        return b

    def scratch(self, name, shape, dt):
        return self.p.dram(name, shape, dt)

    def cast_w(self, st, src, shape):
        p = self.p
        self.ncast += 1
        dst = self.scratch(src.name + "_bf", shape, BF16)
        n = int(np.prod(shape))
        per = n // 128
        names = " ".join(f"d{i}" for i in range(len(shape)))
        sflat = src[:].re(f"{names} -> ({names})").re("(p f) -> p f", p=128)
        dflat = dst[:].re(f"{names} -> ({names})").re("(p f) -> p f", p=128)
        F = 4096
        off = 0
        i = 0
        while off < per:
            w = min(F, per - off)
            a = self.cf32[i % 2]
            b = self.cb16[i % 2]
            p.dma(a[:, 0:w], sflat[:, off:off + w])
            p.copy(b[:, 0:w], a[:, 0:w], eng="pool" if i % 2 == 0 else "act")
            p.dma(dflat[:, off:off + w], b[:, 0:w])
            off += w
            i += 1
        return dst

    def build(self):
        c, p = self.c, self.p
        D, T, NB, KC, NTOK, NBLK = c.D, c.T, c.NB, c.KC, c.NTOK, c.NBLK
        self.glob = ExitStack()
        g = self.glob
        self.ps = [p.ps(g, [128, 512], F32, name=f"psb{i}") for i in range(7)]
        self.psb = p.ps(g, [128, 1024], BF16, name="psbf")
        self.c_ident32 = self.inp("c_ident32", [128, 128])
        self.c_tri64 = self.inp("c_tri64", [64, 64])
        self.c_rmask = self.inp("c_rmask", [128, TB])
        self.c_sbmask = self.inp("c_sbmask", [4, 128, TB])
        self.c_triincl = self.inp("c_triincl", [128, 128])
        self.ident32 = p.sb(g, [128, 128], F32, name="ident32")
        self.ident16 = p.sb(g, [128, 128], BF16, name="ident16")
        self.onesD = p.sb(g, [128, 128], F32, name="onesD")
        self.ones128 = p.sb(g, [128, 128], F32, name="ones128")
        self.negones16 = p.sb(g, [128, 128], BF16, name="negones16")
        self.eps_t = p.sb(g, [128, 1], F32, name="eps_t")
        p.dma(self.ident32[:], self.c_ident32[:])
        p.copy(self.ident16[:], self.ident32[:])
        p.memset(self.onesD[:], 1.0 / D)
        p.memset(self.ones128[:], 1.0 / 128)
        p.memset(self.negones16[:], -1.0)
        p.memset(self.eps_t[:], LN_EPS)
        self.H32 = [self.scratch(f"H32_{i}", [KC, 128, NTOK], F32) for i in range(2)]
        self.H16 = [self.scratch(f"H16_{i}", [KC, 128, NTOK], BF16) for i in range(2)]
        self.zT = [self.scratch(f"zT_{i}", [KC, 128, TB], F32) for i in range(2)]
        self.mixedT = self.scratch("mixedT", [32, 128, NTOK], BF16)
        xT = self.inp("xT", [KC, 128, NTOK])
        outT = self.p.dram("outT", [KC, 128, NTOK], F32, kind="ExternalOutput")
        self.shapes_out = ("outT", (KC, 128, NTOK))

        with ExitStack() as st:
            a = [p.sb(st, [128, 8, TB], F32) for _ in range(2)]
            b16 = [p.sb(st, [128, 8, TB], BF16) for _ in range(2)]
            i = 0
            for b in range(NBLK):
                for gq in range(0, KC, 8):
                    n = min(8, KC - gq)
                    sl = slice(b * TB, (b + 1) * TB)
                    p.dma(a[i % 2][:, 0:n, :], xT[gq:gq + n, :, sl].re("c p t -> p c t"))
                    p.dma(self.H32[0][gq:gq + n, :, sl].re("c p t -> p c t"), a[i % 2][:, 0:n, :])
                    p.copy(b16[i % 2][:, 0:n, :], a[i % 2][:, 0:n, :], eng="pool" if i % 2 else "act")
                    p.dma(self.H16[0][gq:gq + n, :, sl].re("c p t -> p c t"), b16[i % 2][:, 0:n, :])
                    i += 1
        p.barrier()

        self.prep_lb()

        for l in range(c.DEPTH):
            kind = self.kinds[l]
            if kind == "even":
                self.even_layer(l)
            elif kind == "odd":
                self.odd_layer(l)
            self.moe_layer(l)

        with ExitStack() as st:
            a = [p.sb(st, [128, 8, TB], F32) for _ in range(2)]
            i = 0
            for b in range(NBLK):
                for gq in range(0, KC, 8):
                    n = min(8, KC - gq)
                    sl = slice(b * TB, (b + 1) * TB)
                    p.dma(a[i % 2][:, 0:n, :], self.H32[0][gq:gq + n, :, sl].re("c p t -> p c t"))
                    p.dma(outT[gq:gq + n, :, sl].re("c p t -> p c t"), a[i % 2][:, 0:n, :])
                    i += 1
        p.barrier()
        return self

    def prep_lb(self):
        c, p, g = self.c, self.p, self.glob
        DEPTH = c.DEPTH
        raw = self.inp("lb_raw", [128, DEPTH, 16])
        t = p.sb(g, [128, DEPTH, 16], F32, name="lb_t")
        e = p.sb(g, [128, DEPTH, 16], F32, name="lb_e")
        s = p.sb(g, [128, 16], F32, name="lb_s")
        self.lb = p.sb(g, [128, DEPTH, 16], F32, name="lb")
        self.oml = p.sb(g, [128, DEPTH, 16], F32, name="oml")
        p.dma(t[:], raw[:])
        p.act(e[:], t[:], AF.Exp)
        p.copy(s[:], e[:, 0, :])
        for l in range(1, DEPTH):
            p.tt(s[:], s[:], e[:, l, :], ALU.add)
        p.recip(s[:], s[:])
        p.memset(self.lb[:, 0, :], 0.0)
        for l in range(1, DEPTH):
            p.tt(t[:, l, :], e[:, l, :], s[:], ALU.mult)
            p.tt(self.lb[:, l, :], self.lb[:, l - 1, :], t[:, l, :], ALU.add)
        p.ts(self.oml[:], self.lb[:], -1.0, ALU.mult, 1.0, ALU.add)

    def proj_res_ln(self, st, blk, Ablk, KA, wdram, parts, extra, hin, hout, gcol, bcol, ws, hr, zt, zq, psY, psS, psQ, stat):
        c, p = self.c, self.p
        KC = c.KC
        sl = slice(blk * TB, (blk + 1) * TB)
        zT = self.zT[blk % 2]
        kp = KA // parts
        wi = 0
        for n in range(KC):
            ps = psY[n % 2]
            first = True
            for pt in range(parts):
                w = ws[wi % len(ws)]
                wi += 1
                p.dma(w[:, 0:kp * 128], wdram[n, :, pt * kp * 128:(pt + 1) * kp * 128])
                for q in range(kp):
                    last = (pt == parts - 1 and q == kp - 1 and extra is None)
                    p.mm(ps[:], w[:, q * 128:(q + 1) * 128], Ablk[:, pt * kp + q, :], start=first, stop=last)
                    first = False
            if extra is not None:
                extra(n, ps)
            h = hr[n % 2]
            p.dma(h[:], hin[n, :, sl])
            z = zt[n % 2]
            p.stt(z[:], h[:], c.alpha, ps[:], ALU.mult, ALU.add)
            p.dma(zT[n, :, :], z[:])
            p.mm(psS[:], self.onesD[:], z[:], start=(n == 0), stop=(n == KC - 1))
            q2 = zq[n % 2]
            p.act(q2[:], z[:], AF.Square)
            p.mm(psQ[:], self.onesD[:], q2[:], start=(n == 0), stop=(n == KC - 1))
        mean, rstd, nmr, tmp = stat
        p.copy(mean[:], psS[:], eng="act")
        p.tt(tmp[:], mean[:], mean[:], ALU.mult)
        p.tt(tmp[:], psQ[:], tmp[:], ALU.subtract)
        p.act(tmp[:], tmp[:], AF.Sqrt, bias=self.eps_t[:])
        p.recip(rstd[:], tmp[:])
        p.stt(nmr[:], mean[:], -1.0, rstd[:], ALU.mult, ALU.mult)
        for n in range(KC):
            z = zt[n % 2]
            p.dma(z[:], zT[n, :, :])
            q2 = zq[n % 2]
            p.tt(q2[:], z[:], rstd[:], ALU.mult)
            p.tt(q2[:], q2[:], nmr[:], ALU.add)
            h = hr[n % 2]
            p.act(h[:], q2[:], AF.Identity, scale=gcol[:, n:n + 1], bias=bcol[:, n:n + 1])
            p.dma(self.H32[hout][n, :, sl], h[:])
            h16 = self.h16t[n % 2]
            p.copy(h16[:], h[:], eng="pool")
            p.dma(self.H16[hout][n, :, sl], h16[:])

    def even_layer(self, l):
        c, p = self.c, self.p
        D, T, NB, KC, NTOK, NBLK = c.D, c.T, c.NB, c.KC, c.NTOK, c.NBLK
        e = l // 2
        BPS = T // TB
        NCH = T // HG_C
        w_fm = self.inp(f"ev_win_fm{l}", [80, 128, KC * 128])
        w_tm = self.inp(f"ev_win_tm{l}", [16, 128, KC * 256])
        w_out = self.inp(f"ev_wout{l}", [KC, 128, 32 * 128])
        ng_in = self.inp(f"hg_ng{l}", [128, 1])
        ln_in = self.inp(f"ln1_{l}", [128, 2, KC])
        qaT = self.scratch(f"qaT{l}", [NB, 16, 128, T], BF16)
        kaT = self.scratch(f"kaT{l}", [NB, 16, 128, T], BF16)
        vaTM = self.scratch(f"vaTM{l}", [NB, T, 2048], BF16)
        ihTM = self.scratch(f"ihTM{l}", [NB, T, 2048], BF16)
        qbT = self.scratch(f"qbT{l}", [NB, 16, 128, T], BF16)
        ktT = self.scratch(f"ktT{l}", [NB, 16, 128, T], BF16)
        khT = self.scratch(f"khT{l}", [NB, 16, 128, T], BF16)
        sgT = self.scratch(f"sgT{l}", [NB, 16, 128, T], BF16)
        elD = self.scratch(f"elD{l}", [NB, 16, 128, NCH], F32)
        ps = self.ps
        with ExitStack() as st:
            self.cf32 = [p.sb(st, [128, 4096], F32) for _ in range(2)]
            self.cb16 = [p.sb(st, [128, 4096], BF16) for _ in range(2)]
            wfm16 = self.cast_w(st, w_fm, [80, 128, KC * 128])
            wtm16 = self.cast_w(st, w_tm, [16, 128, KC * 256])
            wout16 = self.cast_w(st, w_out, [KC, 128, 32 * 128])
        p.barrier()
        with ExitStack() as st:
            hb = [p.sb(st, [128, KC, TB], BF16) for _ in range(2)]
            ws = [p.sb(st, [128, KC * 128], BF16) for _ in range(3)]
            wt = [p.sb(st, [128, KC * 256], BF16) for _ in range(2)]
            o16 = [p.sb(st, [128, TB], BF16) for _ in range(4)]
            f = {k: p.sb(st, [128, TB], F32, name=f"ev{l}_{k}") for k in
                 ["sq", "sg", "f", "lf", "kin", "cum", "ec", "enc", "kt"]}
            el = p.sb(st, [128, 8], F32)
            rmask = p.sb(st, [128, TB], F32)
            p.dma(rmask[:], self.c_rmask[:])
            wi = 0
            oi = 0
            for b in range(NBLK):
                s, tb = divmod(b, BPS)
                h = hb[b % 2]
                tsl = slice(tb * TB, (tb + 1) * TB)
                p.dma(h[:], self.H16[0][:, :, b * TB:(b + 1) * TB].re("c p t -> p c t"))

                def proj(mc, pst):
                    nonlocal wi
                    w = ws[wi % 3]
                    wi += 1
                    p.dma(w[:], wfm16[mc, :, :])
                    for k in range(KC):
                        p.mm(pst[:], w[:, k * 128:(k + 1) * 128], h[:, k, :], start=(k == 0), stop=(k == KC - 1))

                def out16():
                    nonlocal oi
                    o = o16[oi % 4]
                    oi += 1
                    return o
                for hd in range(16):
                    proj(hd, ps[0])
                    o = out16()
                    p.act(o[:], ps[0][:], AF.Copy, scale=HD ** -0.5)
                    p.dma(qaT[s, hd, :, tsl], o[:])
                    proj(16 + hd, ps[1])
                    o = out16()
                    p.copy(o[:], ps[1][:], eng="dve")
                    p.dma(kaT[s, hd, :, tsl], o[:])
                    proj(32 + hd, ps[2])
                    p.act(f["sq"][:], ps[2][:], AF.Silu)
                    proj(48 + hd, ps[3])
                    p.act(f["sg"][:], ps[3][:], AF.Sigmoid)
                    p.ts(f["f"][:], f["sg"][:], self.oml[:, l, hd:hd + 1], ALU.mult, self.lb[:, l, hd:hd + 1], ALU.add)
                    p.ts(f["lf"][:], f["f"][:], F_MIN, ALU.max)
                    p.act(f["lf"][:], f["lf"][:], AF.Ln)
                    p.ts(f["kin"][:], f["f"][:], -1.0, ALU.mult, 1.0, ALU.add)
                    p.op("dve", lambda E: E.tensor_tensor_scan(out=f["cum"][:].ap, data0=rmask[:].ap, data1=f["lf"][:].ap,
                                                               initial=0.0, op0=ALU.mult, op1=ALU.add),
                         reads=[rmask, f["lf"]], writes=[f["cum"]])
                    p.act(f["ec"][:], f["cum"][:], AF.Exp)
                    p.act(f["enc"][:], f["cum"][:], AF.Exp, scale=-1.0)
                    o = out16()
                    p.tt(o[:], f["sq"][:], f["ec"][:], ALU.mult)
                    p.dma(qbT[s, hd, :, tsl], o[:])
                    p.tt(f["kt"][:], f["kin"][:], f["enc"][:], ALU.mult)
                    o = out16()
                    p.copy(o[:], f["kt"][:], eng="pool")
                    p.dma(ktT[s, hd, :, tsl], o[:])
                    p.copy(el[:], f["ec"][:].re("p (c k) -> p c k", k=HG_C)[:, :, HG_C - 1], eng="pool")
                    p.dma(elD[s, hd, :, tb * 8:(tb + 1) * 8], el[:])
                    o = out16()
                    p.tt(o[:].re("p (c k) -> p c k", k=HG_C), f["kt"][:].re("p (c k) -> p c k", k=HG_C),
                         el[:].un(2).bc([128, 8, HG_C]), ALU.mult)
                    p.dma(khT[s, hd, :, tsl], o[:])
                    proj(64 + hd, ps[4])
                    o = out16()
                    p.act(o[:], ps[4][:], AF.Silu)
                    p.dma(sgT[s, hd, :, tsl], o[:])
                for sb_ in range(16):
                    w = wt[sb_ % 2]
                    p.dma(w[:], wtm16[sb_, :, :])
                    dst = vaTM if sb_ < 8 else ihTM
                    col = (sb_ % 8) * 256
                    for j2 in range(2):
                        pst = ps[5 + (j2 % 2)]
                        for jj in range(2):
                            j = j2 * 2 + jj
                            for k in range(KC):
                                p.mm(pst[:, jj * 256:(jj + 1) * 256], h[:, k, j * 128:(j + 1) * 128],
                                     w[:, k * 256:(k + 1) * 256], start=(k == 0), stop=(k == KC - 1))
                        o = out16()
                        p.copy(o[:], pst[:], eng="act" if j2 else "dve")
                        r0 = tb * TB + j2 * 256
                        p.dma(dst[s, r0:r0 + 256, col:col + 256].re("(j p) n -> p j n", p=128),
                              o[:].re("p (j n) -> p j n", j=2))
        p.barrier()
        self.sb_attention(l, qaT, kaT, vaTM)
        p.barrier()
        self.hgrn(l, qbT, ktT, khT, sgT, elD, ihTM, ng_in)
        p.barrier()
        self.out_proj_ln(l, wout16, ln_in)

    def out_proj_ln(self, l, wout16, ln_in):
        c, p = self.c, self.p
        KC, NBLK = c.KC, c.NBLK
        ps = self.ps
        with ExitStack() as st:
            ab = [p.sb(st, [128, 32, TB], BF16) for _ in range(2)]
            ws = [p.sb(st, [128, 32 * 128], BF16) for _ in range(3)]
            hr = [p.sb(st, [128, TB], F32) for _ in range(2)]
            zt = [p.sb(st, [128, TB], F32) for _ in range(2)]
            zq = [p.sb(st, [128, TB], F32) for _ in range(2)]
            self.h16t = [p.sb(st, [128, TB], BF16) for _ in range(2)]
            stat = [p.sb(st, [128, TB], F32) for _ in range(4)]
            lnp = p.sb(st, [128, 2, KC], F32)
            p.dma(lnp[:], ln_in[:])
            for b in range(NBLK):
                a = ab[b % 2]
                p.dma(a[:], self.mixedT[:, :, b * TB:(b + 1) * TB].re("c p t -> p c t"))
                self.proj_res_ln(st, b, a, 32, wout16, 1, None, self.H32[0], 1, lnp[:, 0, :], lnp[:, 1, :],
                                 ws, hr, zt, zq, [ps[0], ps[1]], ps[2], ps[3], stat)
        p.barrier()

    def sb_attention(self, l, qaT, kaT, vaTM):
        c, p = self.c, self.p
        T, NB = c.T, c.NB
        ps = self.ps
        NT = T // TB
        with ExitStack() as st:
            kT = [p.sb(st, [128, T], BF16) for _ in range(2)]
            vv = [p.sb(st, [128, T // 128, 128], BF16) for _ in range(2)]
            qt = [p.sb(st, [128, TB], BF16) for _ in range(2)]
            e1 = [p.sb(st, [128, TB], F32) for _ in range(2)]
            sp = [p.sb(st, [128, TB], BF16) for _ in range(2)]
            ls32 = p.sb(st, [128, TB], F32)
            ls16 = p.sb(st, [128, TB], BF16)
            wT = [p.sb(st, [128, TB], BF16) for _ in range(2)]
            o16 = [p.sb(st, [128, TB], BF16) for _ in range(2)]
            msk = p.sb(st, [128, 4, TB], BF16)
            mskf = p.sb(st, [128, 4, TB], F32)
            tri = p.sb(st, [128, 128], BF16)
            trif = p.sb(st, [128, 128], F32)
            p.dma(mskf[:], self.c_sbmask[:].re("r s t -> s r t"))
            p.copy(msk[:], mskf[:])
            p.dma(trif[:], self.c_triincl[:])
            p.copy(tri[:], trif[:])
            it = 0
            pi = 0
            for s in range(NB):
                for hd in range(16):
                    k = kT[it % 2]
                    v = vv[it % 2]
                    it += 1
                    p.dma(k[:], kaT[s, hd, :, :])
                    p.dma(v[:], vaTM[s, :, hd * 128:(hd + 1) * 128].re("(c p) d -> p c d", p=128))
                    for i in range(NT):
                        q = qt[i % 2]
                        p.dma(q[:], qaT[s, hd, :, i * TB:(i + 1) * TB])
                        psO = ps[4 + (i % 2)]
                        nchunks = 4 * i + 4
                        for ci, cc in enumerate(range(nchunks - 1, -1, -1)):
                            psA = ps[pi % 2]
                            psB = ps[2 + pi % 2]
                            e = e1[pi % 2]
                            spc = sp[pi % 2]
                            w = wT[pi % 2]
                            pi += 1
                            kc_ = k[:, cc * 128:(cc + 1) * 128]
                            diag = cc >= 4 * i
                            p.mm(psA[:], kc_, q[:])
                            p.act(e[:], psA[:], AF.Exp)
                            p.act(spc[:], e[:], AF.Ln, bias=1.0)
                            if diag:
                                p.tt(spc[:], spc[:], msk[:, cc - 4 * i, :], ALU.mult, eng="pool")
                            p.mm(psB[:], kc_, q[:], start=True, stop=False)
                            p.mm(psB[:], tri[:], spc[:], start=False, stop=(ci == 0))
                            if ci > 0:
                                p.mm(psB[:], self.negones16[:], ls16[:], start=False, stop=True)
                            p.act(w[:], psB[:], AF.Exp)
                            if diag:
                                p.tt(w[:], w[:], msk[:, cc - 4 * i, :], ALU.mult, eng="pool")
                            p.mm(psO[:], v[:, cc, :], w[:], start=(ci == 0), stop=(cc == 0))
                            if cc > 0:
                                if ci == 0:
                                    p.copy(ls32[:], spc[:], eng="dve")
                                else:
                                    p.tt(ls32[:], ls32[:], spc[:], ALU.add)
                                p.copy(ls16[:], ls32[:], eng="pool")
                        o = o16[i % 2]
                        p.copy(o[:], psO[:], eng="dve")
                        p.dma(self.mixedT[hd, :, s * T + i * TB: s * T + (i + 1) * TB], o[:])

    def hgrn(self, l, qbT, ktT, khT, sgT, elD, ihTM, ng_in):
        c, p = self.c, self.p
        T, NB = c.T, c.NB
        ps = self.ps
        NT = T // TB
        NCH = T // HG_C
        G = 8
        with ExitStack() as st:
            def mk(shape, dt, n=1):
                return [[p.sb(st, shape, dt) for _ in range(n)] for _ in range(2)]
            qb = mk([128, G, TB], BF16)
            kt = mk([128, G, TB], BF16)
            kh = mk([128, G, TB], BF16)
            sg = mk([128, G, TB], BF16)
            vv = mk([64, 8, G, 128], BF16)
            el = mk([128, G, 8], F32)
            khTM = [p.sb(st, [64, 8, G, 128], BF16) for _ in range(2)]
            S32 = [p.sb(st, [128, G, 128], F32) for _ in range(2)]
            S16 = [p.sb(st, [128, G, 128], BF16) for _ in range(2)]
            stmp = p.sb(st, [128, G, 128], F32)
            sm = [p.sb(st, [64, G, 64], BF16) for _ in range(2)]
            oall = [p.sb(st, [128, G, TB], F32) for _ in range(2)]
            tri = p.sb(st, [64, 64], F32)
            ng = p.sb(st, [128, 1], F32)
            sq = p.sb(st, [128, TB], F32)
            rs = p.sb(st, [128, TB], F32)
            y1 = p.sb(st, [128, TB], F32)
            o16 = [p.sb(st, [128, TB], BF16) for _ in range(2)]
            p.dma(tri[:], self.c_tri64[:])
            p.dma(ng[:], ng_in[:])
            psS = [ps[0], ps[1]]
            psO = [ps[2], ps[3]]
            for s in range(NB):
                for gi in range(2):
                    p.memset(S32[gi][:], 0.0)
                    p.memset(S16[gi][:], 0.0)
                for i in range(NT):
                    tsl = slice(i * TB, (i + 1) * TB)
                    for gi in range(2):
                        hs = slice(gi * G, (gi + 1) * G)
                        b = 0
                        p.dma(qb[gi][b][:], qbT[s, hs, :, tsl].re("h p t -> p h t"))
                        p.dma(kt[gi][b][:], ktT[s, hs, :, tsl].re("h p t -> p h t"))
                        p.dma(kh[gi][b][:], khT[s, hs, :, tsl].re("h p t -> p h t"))
                        p.dma(sg[gi][b][:], sgT[s, hs, :, tsl].re("h p t -> p h t"))
                        p.dma(el[gi][b][:], elD[s, hs, :, i * 8:(i + 1) * 8].re("h p c -> p h c"))
                        p.dma(vv[gi][b][:], ihTM[s, tsl, gi * G * 128:(gi + 1) * G * 128].re("(c p) (h e) -> p c h e", p=64, e=128))
                        for cq in range(8):
                            for j in range(G):
                                p.tr(self.psb[0:64, j * 128:(j + 1) * 128], kh[gi][b][:, j, cq * 64:(cq + 1) * 64], self.ident16[:])
                            p.copy(khTM[gi][0:64, cq, :, :].re("p h e -> p (h e)"), self.psb[0:64, :], eng="act")
                    for cq in range(8):
                        cs = slice(cq * 64, (cq + 1) * 64)
                        for gi in range(2):
                            b = 0
                            Q, K, Vv = qb[gi][b], kt[gi][b], vv[gi][b]
                            for j in range(G):
                                p.mm(psS[gi][0:64, j * 64:(j + 1) * 64], K[:, j, cs], Q[:, j, cs])
                            p.tt(sm[gi][:], psS[gi][0:64, :].re("p (h t) -> p h t", h=G), tri[:].un(1).bc([64, G, 64]), ALU.mult)
                            for j in range(G):
                                p.mm(psO[gi][:, j * 64:(j + 1) * 64], Vv[0:64, cq, j, :], sm[gi][:, j, :], start=True, stop=False)
                                p.mm(psO[gi][:, j * 64:(j + 1) * 64], S16[gi][:, j, :], Q[:, j, cs], start=False, stop=True)
                            for j in range(G):
                                pst = ps[4 + j // 4]
                                p.mm(pst[:, (j % 4) * 128:(j % 4 + 1) * 128], khTM[gi][0:64, cq, j, :], Vv[0:64, cq, j, :])
                            p.copy(oall[gi][:, :, cs], psO[gi][:].re("p (h t) -> p h t", h=G), eng="act")
                            p.tt(stmp[:], S32[gi][:], el[gi][b][:, :, cq].un(2).bc([128, G, 128]), ALU.mult)
                            for hh in range(2):
                                p.tt(S32[gi][:, hh * 4:(hh + 1) * 4, :], stmp[:, hh * 4:(hh + 1) * 4, :],
                                     ps[4 + hh][:].re("p (h e) -> p h e", h=4), ALU.add)
                            p.copy(S16[gi][:], S32[gi][:], eng="act")
                    for gi in range(2):
                        b = 0
                        for j in range(G):
                            hd = gi * G + j
                            p.act(sq[:], oall[gi][:, j, :], AF.Square)
                            p.mm(ps[6][:], self.ones128[:], sq[:])
                            p.act(rs[:], ps[6][:], AF.Sqrt, bias=self.eps_t[:])
                            p.recip(rs[:], rs[:])
                            p.tt(y1[:], oall[gi][:, j, :], rs[:], ALU.mult)
                            o = o16[j % 2]
                            p.stt(o[:], y1[:], ng[:, 0:1], sg[gi][b][:, j, :], ALU.mult, ALU.mult)
                            p.dma(self.mixedT[16 + hd, :, s * T + i * TB: s * T + (i + 1) * TB], o[:])

    def moe_layer(self, l):
        c, p = self.c, self.p
        D, KC, NBLK = c.D, c.KC, c.NBLK
        ps = self.ps
        w_gu = self.inp(f"moe_wgu{l}", [NE * 6, 128, KC * 128])
        w_dn = self.inp(f"moe_wdn{l}", [KC, 128, 96 * 128])
        b_gu = self.inp(f"moe_bgu{l}", [128, NE, 6])
        b_dn = self.inp(f"moe_bdn{l}", [NE, D])
        w_r = self.inp(f"moe_wr{l}", [128, KC, NE])
        b_r = self.inp(f"moe_br{l}", [NE, 1])
        ln_in = self.inp(f"ln2_{l}", [128, 2, KC])
        with ExitStack() as st:
            self.cf32 = [p.sb(st, [128, 4096], F32) for _ in range(2)]
            self.cb16 = [p.sb(st, [128, 4096], BF16) for _ in range(2)]
            wgu16 = self.cast_w(st, w_gu, [NE * 6, 128, KC * 128])
            wdn16 = self.cast_w(st, w_dn, [KC, 128, 96 * 128])
        p.barrier()
        with ExitStack() as st:
            RN = max(KC * TB + 4 * KC * 128, 3 * 48 * 128)
            R = p.sb(st, [128, RN], BF16, name=f"moeR{l}")
            Rf = V(R, R.t[:].bitcast(F32)) if False else None
            off = 0
            hblk, off = carve(R, off, [128, KC, TB], 2, "hblk")
            wg = []
            for i in range(4):
                b_, off = carve(R, off, [128, KC * 128], 2, f"wg{i}")
                wg.append(b_)
            gu_bufs = [hblk] + wg
            off2 = 0
            wd = []
            for i in range(3):
                b_, off2 = carve(R, off2, [128, 48 * 128], 2, f"wd{i}")
                wd.append(b_)
            dn_bufs = list(wd)
            for a_ in gu_bufs:
                a_.aliases = list(dn_bufs)
            for a_ in dn_bufs:
                a_.aliases = list(gu_bufs)
            actT = p.sb(st, [128, 96, TB], BF16)
            t = {k: p.sb(st, [128, TB], F32, name=f"moe{l}_{k}") for k in ["glu", "sig", "lin", "a1"]}
            hr = [p.sb(st, [128, TB], F32) for _ in range(2)]
            zt = [t["glu"], t["sig"]]
            zq = [t["lin"], t["a1"]]
            self.h16t = [p.sb(st, [128, TB], BF16) for _ in range(2)]
            stat = [p.sb(st, [128, TB], F32) for _ in range(4)]
            lnp = p.sb(st, [128, 2, KC], F32)
            bgu = p.sb(st, [128, NE, 6], F32)
            wr = p.sb(st, [128, KC, NE], F32)
            br = p.sb(st, [NE, 1], F32)
            lgT = p.sb(st, [NE, TB], F32)
            lg = p.sb(st, [128, 4, NE], F32)
            gt = p.sb(st, [128, 4, NE], F32)
            m8 = p.sb(st, [128, 8], F32)
            sc = p.sb(st, [128, 4], F32)
            gT32 = p.sb(st, [NE, TB], F32)
            bdc = [p.sb(st, [NE, 128], F32) for _ in range(2)]
            p.dma(lnp[:], ln_in[:])
            p.dma(bgu[:], b_gu[:])
            p.dma(wr[:], w_r[:])
            p.dma(br[:], b_r[:])
            wi = 0
            for b in range(NBLK):
                sl = slice(b * TB, (b + 1) * TB)
                p.dma(hblk[:], self.H16[1][:, :, sl].re("c p t -> p c t"))
                for k in range(KC):
                    h = hr[k % 2]
                    p.dma(h[:], self.H32[1][k, :, sl])
                    p.mm(ps[6][0:NE, :], wr[:, k, :], h[:], start=(k == 0), stop=(k == KC - 1))
                p.act(lgT[:], ps[6][0:NE, :], AF.Identity, bias=br[:, 0:1])
                for j in range(4):
                    p.tr(ps[5][:, j * NE:(j + 1) * NE], lgT[:, j * 128:(j + 1) * 128], self.ident32[0:NE, 0:NE])
                p.copy(lg[:].re("p j e -> p (j e)"), ps[5][:, 0:4 * NE], eng="act")
                for j in range(4):
                    p.op("dve", lambda E: E.max(out=m8[:].ap, in_=lg[:, j, :].ap), reads=[lg], writes=[m8])
                    p.ts(gt[:, j, :], lg[:, j, :], m8[:, 3:4], ALU.is_ge)
                    p.ts(sc[:, 0:1], m8[:, 0:1], -1.0, ALU.mult)
                    p.act(lg[:, j, :], lg[:, j, :], AF.Exp, bias=sc[:, 0:1])
                    p.tt(gt[:, j, :], gt[:, j, :], lg[:, j, :], ALU.mult)
                    p.op("dve", lambda E: E.tensor_reduce(out=sc[:, 1:2].ap, in_=gt[:, j, :].ap, axis=AX.X, op=ALU.add),
                         reads=[gt], writes=[sc])
                    p.recip(sc[:, 2:3], sc[:, 1:2])
                    p.ts(gt[:, j, :], gt[:, j, :], sc[:, 2:3], ALU.mult)
                    p.tr(ps[5][0:NE, j * 128:(j + 1) * 128], gt[:, j, :], self.ident32[:])
                p.copy(gT32[:], ps[5][0:NE, :], eng="act")
                for e in range(NE):
                    p.mm(ps[4][:], self.ident32[0:NE, e:e + 1].bc([NE, 128]), gT32[:])
                    for cc in range(3):
                        wgs = wg[wi % 4]
                        wls = wg[(wi + 1) % 4]
                        wi += 2
                        p.dma(wgs[:], wgu16[e * 6 + cc, :, :])
                        p.dma(wls[:], wgu16[e * 6 + 3 + cc, :, :])
                        pg = ps[(cc % 2) * 2]
                        pl = ps[(cc % 2) * 2 + 1]
                        for k in range(KC):
                            p.mm(pg[:], wgs[:, k * 128:(k + 1) * 128], hblk[:, k, :], start=(k == 0), stop=(k == KC - 1))
                        for k in range(KC):
                            p.mm(pl[:], wls[:, k * 128:(k + 1) * 128], hblk[:, k, :], start=(k == 0), stop=(k == KC - 1))
                        p.ts(t["glu"][:], pg[:], bgu[:, e, cc:cc + 1], ALU.add, LIM, ALU.min)
                        p.act(t["sig"][:], t["glu"][:], AF.Sigmoid, scale=SW_ALPHA)
                        p.ts(t["lin"][:], pl[:], bgu[:, e, 3 + cc:4 + cc], ALU.add, LIM, ALU.min)
                        p.ts(t["lin"][:], t["lin"][:], -LIM, ALU.max, 1.0, ALU.add)
                        p.tt(t["a1"][:], t["glu"][:], t["sig"][:], ALU.mult)
                        p.tt(t["a1"][:], t["a1"][:], t["lin"][:], ALU.mult, eng="pool")
                        p.tt(actT[:, e * 3 + cc, :], t["a1"][:], ps[4][:], ALU.mult)

                def extra(n, pst):
                    bb = bdc[n % 2]
                    p.dma(bb[:], b_dn[:, n * 128:(n + 1) * 128])
                    p.mm(pst[:], bb[:], gT32[:], start=False, stop=True)
                self.proj_res_ln(st, b, actT, 96, wdn16, 2, extra, self.H32[1], 0, lnp[:, 0, :], lnp[:, 1, :],
                                 wd, hr, zt, zq, [ps[0], ps[1]], ps[2], ps[3], stat)
        p.barrier()


def host_consts(cfg):
    tri64 = (np.arange(64)[:, None] <= np.arange(64)[None, :]).astype(np.float32)
    rmask = np.ones((128, TB), np.float32)
    rmask[:, ::HG_C] = 0.0
    sbm = np.zeros((4, 128, TB), np.float32)
    for r in range(4):
        sbm[r] = ((r * 128 + np.arange(128))[:, None] < np.arange(TB)[None, :])
    triincl = -(np.arange(128)[:, None] >= np.arange(128)[None, :]).astype(np.float32)
    return {"c_ident32": np.eye(128, dtype=np.float32), "c_tri64": tri64, "c_rmask": rmask,
            "c_sbmask": sbm, "c_triincl": triincl}


def host_inputs(cfg, inp, core, kinds):
    D, T, NB, KC = cfg.D, cfg.T, cfg.NB, cfg.KC
    m = dict(host_consts(cfg))
    x = np.asarray(inp["x"])[core * NB:(core + 1) * NB].reshape(NB * T, D)
    m["xT"] = np.ascontiguousarray(x.T).reshape(KC, 128, NB * T)
    lbr = np.asarray(inp["hg_lb_raw"])
    m["lb_raw"] = np.ascontiguousarray(lbr.reshape(cfg.DEPTH, 16, 128).transpose(2, 0, 1))
    for l in range(cfg.DEPTH):
        if kinds[l] == "even":
            e = l // 2
            w = np.asarray(inp["ev_w_in"][e])
            cols = lambda a, b: w[:, a * 2048:b * 2048]
            fm = np.concatenate([cols(0, 1), cols(1, 2), cols(3, 4), cols(4, 5), cols(6, 7)], axis=1)
            tm = np.concatenate([cols(2, 3), cols(5, 6)], axis=1)
            m[f"ev_win_fm{l}"] = fm_layout(fm).reshape(80, 128, KC * 128)
            m[f"ev_win_tm{l}"] = tm_layout(tm, 256).reshape(16, 128, KC * 256)
            m[f"ev_wout{l}"] = fm_layout(np.asarray(inp["ev_w_out"][e])).reshape(KC, 128, 32 * 128)
            m[f"hg_ng{l}"] = np.asarray(inp["hg_norm_g"][e]).reshape(128, 1)
        m[f"ln1_{l}"] = np.ascontiguousarray(np.stack([pvec(np.asarray(inp["ln1_g"][l])), pvec(np.asarray(inp["ln1_b"][l]))], 1))
        m[f"ln2_{l}"] = np.ascontiguousarray(np.stack([pvec(np.asarray(inp["ln2_g"][l])), pvec(np.asarray(inp["ln2_b"][l]))], 1))
        wgu = np.asarray(inp["exp_w_gu"][l])
        wgu = wgu.reshape(NE, KC, 128, EFF, 2).transpose(0, 4, 3, 1, 2)
        wgu = wgu.reshape(NE, 2, 3, 128, KC, 128)
        wgu = wgu.transpose(0, 1, 2, 5, 4, 3)
        m[f"moe_wgu{l}"] = np.ascontiguousarray(wgu).reshape(NE * 6, 128, KC * 128)
        bgu = np.asarray(inp["exp_b_gu"][l]).reshape(NE, EFF, 2).transpose(0, 2, 1).reshape(NE, 6, 128)
        m[f"moe_bgu{l}"] = np.ascontiguousarray(bgu.transpose(2, 0, 1))
        wd = np.asarray(inp["exp_w_down"][l]).reshape(NE, 3, 128, KC, 128)
        m[f"moe_wdn{l}"] = np.ascontiguousarray(wd.transpose(3, 2, 0, 1, 4)).reshape(KC, 128, 96 * 128)
        m[f"moe_bdn{l}"] = np.ascontiguousarray(np.asarray(inp["exp_b_down"][l]))
        m[f"moe_wr{l}"] = np.ascontiguousarray(np.asarray(inp["router_w"][l]).reshape(KC, 128, NE).transpose(1, 0, 2))
        m[f"moe_br{l}"] = np.asarray(inp["router_b"][l]).reshape(NE, 1)
    return m


_CACHE = {}


def run_model(cfg, inp, kinds=None):
    kinds = kinds or ["even" if l % 2 == 0 else "odd" for l in range(cfg.DEPTH)]
    key = (cfg.D, cfg.T, cfg.NB, cfg.NCORE, cfg.DEPTH, tuple(kinds))
    if key not in _CACHE:
        _CACHE[key] = Builder(cfg, kinds).build()
    bld = _CACHE[key]
    shared = None
    in_maps = []
    for core in range(cfg.NCORE):
        if shared is None:
            m = host_inputs(cfg, inp, core, kinds)
            m = {k: np.ascontiguousarray(v, dtype=np.float32).reshape(bld.shapes[k]) for k, v in m.items() if k in bld.shapes}
            assert set(m) == set(bld.shapes), (set(bld.shapes) - set(m), set(m) - set(bld.shapes))
            shared = m
        else:
            m = dict(shared)
            x = np.asarray(inp["x"])[core * cfg.NB:(core + 1) * cfg.NB].reshape(cfg.NTOK, cfg.D)
            m["xT"] = np.ascontiguousarray(x.T, dtype=np.float32).reshape(bld.shapes["xT"])
        in_maps.append(m)
    res = run_bass_kernel_spmd(bld.p.nc, in_maps, core_ids=list(range(cfg.NCORE)))
    outs = []
    for core in range(cfg.NCORE):
        oT = np.asarray(res.results[core]["outT"]).reshape(cfg.D, cfg.NTOK)
        outs.append(np.ascontiguousarray(oT.T).reshape(cfg.NB, cfg.T, cfg.D))
    return np.concatenate(outs, 0)


NCORE_USED = 4


def kernel(**inputs):
    B, T, D = inputs["x"].shape
    ncore = NCORE_USED
    cfg = Cfg(D=D, T=T, NB=B // ncore, NCORE=ncore, DEPTH=inputs["ln1_g"].shape[0])
    return run_model(cfg, inputs).astype(np.float32)


CMP_LEN, CMP_STRIDE, SLC, NSEL, WIN = 32, 16, 64, 16, 512
OFF_S = 511
LS = 1536
OFF_C = 2063
LC = 4608
NEG = -1e30
FORCED = 1e9


def _bucket(dist):
    dist = np.maximum(dist, 0)
    r = np.log(np.maximum(dist, 16).astype(np.float32) / np.float32(16)) / np.float32(math.log(128 / 16))
    large = np.minimum(16 + (r * np.float32(16)).astype(np.int32), 31)
    return np.where(dist < 16, dist, large)


def nsa_consts(cfg):
    T = cfg.T
    m = {}
    d = np.arange(LS) - OFF_S
    oh = np.zeros((32, LS), np.float32)
    ok = d >= 0
    oh[_bucket(d)[ok], np.arange(LS)[ok]] = 1.0
    m["c_ohS"] = oh
    ohw = oh.copy()
    ohw[:, d >= WIN] = 0.0
    m["c_ohW"] = ohw
    d = np.arange(LC) - OFF_C
    oh = np.zeros((32, LC), np.float32)
    ok = d >= 0
    oh[_bucket(d)[ok], np.arange(LC)[ok]] = 1.0
    m["c_ohC"] = oh
    m["c_antiI"] = np.eye(128, dtype=np.float32)[::-1].copy()
    ncmp = (T - CMP_LEN) // CMP_STRIDE + 1
    nslc = T // SLC
    cs = np.arange(ncmp) * CMP_STRIDE
    ce = cs + CMP_LEN - 1
    ss = np.arange(nslc) * SLC
    ov = ((cs[:, None] < ss[None, :] + SLC) & (ce[:, None] >= ss[None, :])).astype(np.float32)
    nch = (ncmp + 127) // 128
    ovp = np.zeros((nch * 128, nslc), np.float32)
    ovp[:ncmp] = ov
    m["c_overlap"] = np.ascontiguousarray(ovp.reshape(nch, 128, nslc).transpose(1, 0, 2))
    NT = T // TB
    sm = np.zeros((NT, 3, 128, 4, nslc), np.float32)
    for i in range(NT):
        t = i * TB + (np.arange(4)[None, :] * 128 + np.arange(128)[:, None])
        qb = t // SLC
        j = np.arange(nslc)[None, None, :]
        valid = j <= qb[:, :, None]
        forced = (j == 0) | (j == qb[:, :, None]) | (j == qb[:, :, None] - 1)
        sm[i, 0] = (valid & ~forced)
        sm[i, 1] = np.where(valid & forced, FORCED, np.where(valid, 0.0, NEG))
        sm[i, 2] = valid
    m["c_selmask"] = sm
    m["c_expand"] = (np.arange(nslc)[:, None] == (np.arange(T)[None, :] // SLC)).astype(np.float32)
    return m


def _odd_layer(self, l):
    c, p = self.c, self.p
    D, T, NB, KC, NTOK, NBLK = c.D, c.T, c.NB, c.KC, c.NTOK, c.NBLK
    BPS = T // TB
    NT = T // TB
    NCMP = (T - CMP_LEN) // CMP_STRIDE + 1
    NCC = (NCMP + 127) // 128
    NSLC = T // SLC
    ps = self.ps
    w_fm = self.inp(f"od_win_fm{l}", [49, 128, KC * 128])
    w_tm = self.inp(f"od_win_tm{l}", [4, 128, KC * 256])
    w_out = self.inp(f"od_wout{l}", [KC, 128, 32 * 128])
    ln_in = self.inp(f"ln1_{l}", [128, 2, KC])
    cw1 = [self.inp(f"cmp_w1_{l}_{i}", [128, 32, 128]) for i in range(2)]
    cw2 = [self.inp(f"cmp_w2_{l}_{i}", [128, 128]) for i in range(2)]
    cpos = [self.inp(f"cmp_pos_{l}_{i}", [128, 32]) for i in range(2)]
    qT = self.scratch(f"qT{l}", [NB, 32, 128, T], BF16)
    kcT = self.scratch(f"kcT{l}", [NB, 4, 128, T], BF16)
    vcT = self.scratch(f"vcT{l}", [NB, 4, 128, T], BF16)
    ksT = self.scratch(f"ksT{l}", [NB, 4, 128, T], BF16)
    kwT = self.scratch(f"kwT{l}", [NB, 4, 128, T], BF16)
    gTd = self.scratch(f"gTd{l}", [NB, 128, T], F32)
    vsTM = self.scratch(f"vsTM{l}", [NB, T, 512], BF16)
    vwTM = self.scratch(f"vwTM{l}", [NB, T, 512], BF16)
    ocT = self.scratch(f"ocT{l}", [8, 128, T], F32)
    with ExitStack() as st:
        self.cf32 = [p.sb(st, [128, 4096], F32) for _ in range(2)]
        self.cb16 = [p.sb(st, [128, 4096], BF16) for _ in range(2)]
        wfm16 = self.cast_w(st, w_fm, [49, 128, KC * 128])
        wtm16 = self.cast_w(st, w_tm, [4, 128, KC * 256])
        wout16 = self.cast_w(st, w_out, [KC, 128, 32 * 128])
    p.barrier()
    if not hasattr(self, "nsa_M"):
        self.nsa_prep()
    with ExitStack() as st:
        hb = [p.sb(st, [128, KC, TB], BF16) for _ in range(2)]
        ws = [p.sb(st, [128, KC * 128], BF16) for _ in range(3)]
        wt = [p.sb(st, [128, KC * 256], BF16) for _ in range(2)]
        o16 = [p.sb(st, [128, TB], BF16) for _ in range(4)]
        o32 = [p.sb(st, [128, TB], F32) for _ in range(2)]
        wi = 0
        oi = 0
        for b in range(NBLK):
            s, tb = divmod(b, BPS)
            h = hb[b % 2]
            tsl = slice(tb * TB, (tb + 1) * TB)
            p.dma(h[:], self.H16[0][:, :, b * TB:(b + 1) * TB].re("c p t -> p c t"))
            for mc in range(49):
                w = ws[wi % 3]
                wi += 1
                pst = ps[mc % 4]
                p.dma(w[:], wfm16[mc, :, :])
                for k in range(KC):
                    p.mm(pst[:], w[:, k * 128:(k + 1) * 128], h[:, k, :], start=(k == 0), stop=(k == KC - 1))
                if mc == 48:
                    o = o32[b % 2]
                    p.act(o[:], pst[:], AF.Sigmoid)
                    p.dma(gTd[s, :, tsl], o[:])
                    continue
                o = o16[oi % 4]
                oi += 1
                if mc < 32:
                    p.act(o[:], pst[:], AF.Copy, scale=HD ** -0.5)
                    p.dma(qT[s, mc, :, tsl], o[:])
                else:
                    p.copy(o[:], pst[:], eng="dve" if mc % 2 else "act")
                    dst = [kcT, vcT, ksT, kwT][(mc - 32) // 4]
                    p.dma(dst[s, (mc - 32) % 4, :, tsl], o[:])
            for sb_ in range(4):
                w = wt[sb_ % 2]
                p.dma(w[:], wtm16[sb_, :, :])
                dst = vsTM if sb_ < 2 else vwTM
                col = (sb_ % 2) * 256
                for j2 in range(2):
                    pst = ps[4 + (j2 % 2)]
                    for jj in range(2):
                        j = j2 * 2 + jj
                        for k in range(KC):
                            p.mm(pst[:, jj * 256:(jj + 1) * 256], h[:, k, j * 128:(j + 1) * 128],
                                 w[:, k * 256:(k + 1) * 256], start=(k == 0), stop=(k == KC - 1))
                    o = o16[oi % 4]
                    oi += 1
                    p.copy(o[:], pst[:], eng="act" if j2 else "dve")
                    r0 = tb * TB + j2 * 256
                    p.dma(dst[s, r0:r0 + 256, col:col + 256].re("(j p) n -> p j n", p=128), o[:].re("p (j n) -> p j n", j=2))
    p.barrier()
    Ms, Mw, Mc, b31 = self.nsa_M
    with ExitStack() as st:
        w1 = [p.sb(st, [128, 32, 128], BF16) for _ in range(2)]
        w2 = [p.sb(st, [128, 128], BF16) for _ in range(2)]
        posT = [p.sb(st, [128, 32], BF16) for _ in range(2)]
        cvec = [p.sb(st, [128, 1], F32) for _ in range(2)]
        tmpf = p.sb(st, [128, 32, 128], F32)
        ones16 = p.sb(st, [128, 128], BF16)
        p.memset(ones16[:], 1.0)
        for i in range(2):
            p.dma(tmpf[:], cw1[i][:])
            p.copy(w1[i][:], tmpf[:])
            p.dma(tmpf[:, 0, :], cw2[i][:])
            p.copy(w2[i][:], tmpf[:, 0, :])
            p.dma(tmpf[:, 1, 0:32], cpos[i][:])
            p.copy(posT[i][:], tmpf[:, 1, 0:32])
            for ll in range(32):
                p.mm(ps[6][:, 0:1], w1[i][:, ll, :], posT[i][:, ll:ll + 1], start=(ll == 0), stop=(ll == 31))
            p.copy(cvec[i][:], ps[6][:, 0:1], eng="act")
        ovl = p.sb(st, [128, NCC, NSLC], BF16)
        ovf = p.sb(st, [128, NCC, NSLC], F32)
        p.dma(ovf[:], self.c_overlap[:])
        p.copy(ovl[:], ovf[:])
        expd = p.sb(st, [NSLC, T], BF16)
        for q0 in range(0, T, 2048):
            q1 = min(T, q0 + 2048)
            p.dma(tmpf[0:NSLC, :, :].re("p a b -> p (a b)")[:, 0:q1 - q0], self.c_expand[:, q0:q1])
            p.copy(expd[:, q0:q1], tmpf[0:NSLC, :, :].re("p a b -> p (a b)")[:, 0:q1 - q0])
        selm = p.sb(st, [128, 3, 4, NSLC], F32)
        xin = p.sb(st, [128, T], BF16)
        hid = p.sb(st, [128, NCC * 128], BF16)
        kcmp = p.sb(st, [128, NCC * 128], BF16)
        vcmp = p.sb(st, [128, NCC, 128], BF16)
        kst = p.sb(st, [128, T], BF16)
        kwt = p.sb(st, [128, T], BF16)
        vst = p.sb(st, [128, T // 128, 128], BF16)
        vwt = p.sb(st, [128, T // 128, 128], BF16)
        qg = [p.sb(st, [128, 8, TB], BF16) for _ in range(2)]
        q1h = [p.sb(st, [128, TB], BF16) for _ in range(2)]
        gt32 = [p.sb(st, [128, TB], F32) for _ in range(2)]
        selT = p.sb(st, [NSLC, T], BF16)
        ee = [p.sb(st, [128, TB], BF16) for _ in range(3)]
        pT = [p.sb(st, [128, TB], BF16) for _ in range(4)]
        mt = [p.sb(st, [128, TB], BF16) for _ in range(4)]
        msel = [p.sb(st, [128, 5, TB], BF16) for _ in range(1)]
        mwin = [p.sb(st, [128, 8, TB], BF16) for _ in range(1)]
        rz = p.sb(st, [128, TB], F32)
        fac = p.sb(st, [128, TB], F32)
        acc = p.sb(st, [128, TB], F32)
        tmp = p.sb(st, [128, TB], F32)
        o16 = [p.sb(st, [128, TB], BF16) for _ in range(2)]
        impT = p.sb(st, [NSLC, TB], F32)
        imp = p.sb(st, [128, 4, NSLC], F32)
        imp2 = p.sb(st, [128, 4, NSLC], F32)
        selt = p.sb(st, [128, 4, NSLC], F32)
        m8 = p.sb(st, [128, 8], F32)
        ei = 0
        pi = 0
        mi = 0

        def nxt(lst, idx):
            return lst[idx % len(lst)]
        for s in range(NB):
            for g in range(4):
                for i, src in enumerate([kcT, vcT]):
                    p.dma(xin[:], src[s, g, :, :])
                    for ll in range(32):
                        p.mm(ps[5][:, 0:NCMP], w1[i][:, ll, :], xin[:, ll:ll + CMP_STRIDE * (NCMP - 1) + 1:CMP_STRIDE],
                             start=(ll == 0), stop=(ll == 31))
                    p.act(hid[:, 0:NCMP], ps[5][:, 0:NCMP], AF.Silu, bias=cvec[i][:, 0:1])
                    if i == 0:
                        p.mm(ps[5][:, 0:NCMP], w2[i][:], hid[:, 0:NCMP])
                        p.copy(kcmp[:, 0:NCMP], ps[5][:, 0:NCMP], eng="act")
                    else:
                        for ch in range(NCC):
                            nn = min(128, NCMP - ch * 128)
                            p.mm(ps[5][0:nn, 0:128], hid[:, ch * 128:ch * 128 + nn], w2[i][:])
                            p.copy(vcmp[0:nn, ch, :], ps[5][0:nn, 0:128], eng="act")
                p.dma(kst[:], ksT[s, g, :, :])
                p.dma(kwt[:], kwT[s, g, :, :])
                p.dma(vst[:], vsTM[s, :, g * 128:(g + 1) * 128].re("(c p) d -> p c d", p=128))
                p.dma(vwt[:], vwTM[s, :, g * 128:(g + 1) * 128].re("(c p) d -> p c d", p=128))
                for i in range(NT):
                    tsl = slice(i * TB, (i + 1) * TB)
                    qq = qg[i % 2]
                    gg = gt32[i % 2]
                    p.dma(qq[:], qT[s, g * 8:(g + 1) * 8, :, tsl].re("h p t -> p h t"))
                    p.dma(gg[:], gTd[s, :, tsl])
                    p.dma(selm[:], self.c_selmask[i].re("a p j m -> p a j m"))
                    nmax = (i * TB + TB - 1 - (CMP_LEN - 1)) // CMP_STRIDE
                    chs = [ch for ch in range(NCC) if ch * 128 <= min(nmax, NCMP - 1)]
                    for r in range(8):
                        hh = g * 8 + r
                        pts = []
                        for ci, ch in enumerate(chs):
                            nn = min(128, NCMP - ch * 128)
                            delta = i - 4 * ch
                            pa = ps[pi % 2]
                            pi += 1
                            p.mm(pa[0:nn, :], kcmp[:, ch * 128:ch * 128 + nn], qq[:, r, :])
                            pt = nxt(pT, ei)
                            ei += 1
                            if delta <= 4:
                                e = nxt(ee, ei)
                                m_ = nxt(mt, mi)
                                mi += 1
                                p.dma(m_[:], Mc[delta][hh, :, :])
                                p.act(e[0:nn, :], pa[0:nn, :], AF.Exp)
                                p.tt(pt[0:nn, :], e[0:nn, :], m_[0:nn, :], ALU.mult, eng="pool")
                            else:
                                p.act(pt[0:nn, :], pa[0:nn, :], AF.Exp, bias=b31[0:nn, hh:hh + 1])
                            p.mm(ps[2][:], vcmp[0:nn, ch, :], pt[0:nn, :], start=(ci == 0), stop=(ci == len(chs) - 1))
                            p.mm(ps[3][:], ones16[0:nn, :], pt[0:nn, :], start=(ci == 0), stop=(ci == len(chs) - 1))
                            pts.append((pt, nn, ch))
                        p.ts(rz[:], ps[3][:], 1e-30, ALU.max)
                        p.recip(rz[:], rz[:])
                        p.mm(ps[5][:], self.ident32[0:96, g * 24 + r * 3:g * 24 + r * 3 + 1].bc([96, 128]), gg[0:96, :])
                        p.tt(fac[:], rz[:], ps[5][:], ALU.mult)
                        p.tt(acc[:], ps[2][:], fac[:], ALU.mult)
                        p.dma(ocT[r, :, tsl], acc[:])
                        for ci, (pt, nn, ch) in enumerate(pts):
                            p.tt(pt[0:nn, :], pt[0:nn, :], rz[0:nn, :], ALU.mult, eng="pool")
                            p.mm(ps[4][0:NSLC, :], ovl[0:nn, ch, :], pt[0:nn, :],
                                 start=(r == 0 and ci == 0), stop=(r == 7 and ci == len(pts) - 1))
                    p.copy(impT[:], ps[4][0:NSLC, :], eng="act")
                    for j in range(4):
                        p.tr(ps[6][:, j * NSLC:(j + 1) * NSLC], impT[:, j * 128:(j + 1) * 128], self.ident32[0:NSLC, 0:NSLC])
                    p.tt(imp[:].re("p j m -> p (j m)"), ps[6][:, 0:4 * NSLC], selm[:, 0, :, :].re("p j m -> p (j m)"), ALU.mult)
                    p.tt(imp[:], imp[:], selm[:, 1, :, :], ALU.add)
                    for j in range(4):
                        p.op("dve", lambda E: E.max(out=m8[:].ap, in_=imp[:, j, :].ap), reads=[imp], writes=[m8])
                        p.op("dve", lambda E: E.match_replace(out=imp2[:, j, :].ap, in_to_replace=m8[:].ap, in_values=imp[:, j, :].ap,
                                                              imm_value=-3e38), reads=[m8, imp], writes=[imp2])
                        p.op("dve", lambda E: E.max(out=m8[:].ap, in_=imp2[:, j, :].ap), reads=[imp2], writes=[m8])
                        p.ts(selt[:, j, :], imp[:, j, :], m8[:, 7:8], ALU.is_ge)
                    p.tt(selt[:], selt[:], selm[:, 2, :, :], ALU.mult)
                    for j in range(4):
                        p.tr(ps[6][0:NSLC, j * 128:(j + 1) * 128], selt[:, j, :], self.ident32[:])
                    p.copy(selT[:, tsl], ps[6][0:NSLC, :], eng="act")
                for r in range(8):
                    hh = g * 8 + r
                    p.dma(msel[0][:], Ms[:, hh, :, :].re("a p t -> p a t"))
                    p.dma(mwin[0][:], Mw[:, hh, :, :].re("a p t -> p a t"))
                    for i in range(NT):
                        tsl = slice(i * TB, (i + 1) * TB)
                        q1 = q1h[i % 2]
                        gg = gt32[i % 2]
                        p.dma(q1[:], qT[s, hh, :, tsl])
                        p.dma(gg[:], gTd[s, :, tsl])
                        p.dma(acc[:], ocT[r, :, tsl])
                        ncc = 4 * i + 4
                        for cc in range(ncc):
                            rel = 4 * i - cc
                            pa = ps[pi % 2]
                            pi += 1
                            p.mm(pa[:], kst[:, cc * 128:(cc + 1) * 128], q1[:])
                            p.mm(ps[5][:], expd[:, cc * 128:(cc + 1) * 128], selT[:, tsl])
                            e = nxt(ee, ei)
                            pt = nxt(pT, ei)
                            ei += 1
                            if rel >= 2:
                                p.act(e[:], pa[:], AF.Exp, bias=b31[:, hh:hh + 1])
                            else:
                                p.act(e[:], pa[:], AF.Exp)
                                p.tt(e[:], e[:], msel[0][:, rel + 3, :], ALU.mult, eng="pool")
                            p.tt(pt[:], e[:], ps[5][:], ALU.mult)
                            p.mm(ps[2][:], vst[:, cc, :], pt[:], start=(cc == 0), stop=(cc == ncc - 1))
                            p.mm(ps[3][:], ones16[:], pt[:], start=(cc == 0), stop=(cc == ncc - 1))
                        p.recip(rz[:], ps[3][:])
                        p.mm(ps[6][:], self.ident32[0:96, g * 24 + r * 3 + 1:g * 24 + r * 3 + 2].bc([96, 128]), gg[0:96, :])
                        p.tt(fac[:], rz[:], ps[6][:], ALU.mult)
                        p.tt(tmp[:], ps[2][:], fac[:], ALU.mult)
                        p.tt(acc[:], acc[:], tmp[:], ALU.add)
                        c0 = max(0, 4 * i - 4)
                        for cc in range(c0, ncc):
                            rel = 4 * i - cc
                            pa = ps[pi % 2]
                            pi += 1
                            p.mm(pa[:], kwt[:, cc * 128:(cc + 1) * 128], q1[:])
                            e = nxt(ee, ei)
                            pt = nxt(pT, ei)
                            ei += 1
                            p.act(e[:], pa[:], AF.Exp)
                            p.tt(pt[:], e[:], mwin[0][:, rel + 3, :], ALU.mult, eng="pool" if cc % 2 else "dve")
                            p.mm(ps[2][:], vwt[:, cc, :], pt[:], start=(cc == c0), stop=(cc == ncc - 1))
                            p.mm(ps[3][:], ones16[:], pt[:], start=(cc == c0), stop=(cc == ncc - 1))
                        p.recip(rz[:], ps[3][:])
                        p.mm(ps[6][:], self.ident32[0:96, g * 24 + r * 3 + 2:g * 24 + r * 3 + 3].bc([96, 128]), gg[0:96, :])
                        p.tt(fac[:], rz[:], ps[6][:], ALU.mult)
                        p.tt(tmp[:], ps[2][:], fac[:], ALU.mult)
                        o = o16[i % 2]
                        p.tt(o[:], acc[:], tmp[:], ALU.add)
                        p.dma(self.mixedT[hh, :, s * T + i * TB:s * T + (i + 1) * TB], o[:])
    p.barrier()
    self.out_proj_ln(l, wout16, ln_in)


def _nsa_prep(self):
    c, p = self.c, self.p
    ps = self.ps
    g = self.glob
    rb = self.inp("rel_bias", [32, 32])
    ohS = self.inp("c_ohS", [32, LS])
    ohW = self.inp("c_ohW", [32, LS])
    ohC = self.inp("c_ohC", [32, LC])
    antiI = self.inp("c_antiI", [128, 128])
    self.c_overlap = self.inp("c_overlap", [128, (((c.T - CMP_LEN) // CMP_STRIDE + 1) + 127) // 128, c.T // SLC])
    self.c_selmask = self.inp("c_selmask", [c.T // TB, 3, 128, 4, c.T // SLC])
    self.c_expand = self.inp("c_expand", [c.T // SLC, c.T])
    vS = self.scratch("vecS", [32, LS], F32)
    vW = self.scratch("vecW", [32, LS], F32)
    vC = self.scratch("vecC", [32, LC], F32)
    Ms = self.scratch("M_s", [5, 32, 128, TB], BF16)
    Mw = self.scratch("M_w", [8, 32, 128, TB], BF16)
    Mc = self.scratch("M_c", [5, 32, 128, TB], BF16)
    b31 = p.sb(g, [128, 32], F32, name="b31")
    with ExitStack() as st:
        tab = p.sb(st, [32, 32], F32)
        eb = p.sb(st, [32, 32], F32)
        oh = [p.sb(st, [32, TB], F32) for _ in range(2)]
        vo = [p.sb(st, [32, TB], F32) for _ in range(2)]
        J = p.sb(st, [128, 128], F32)
        tl = [p.sb(st, [128, TB], F32) for _ in range(2)]
        to = [p.sb(st, [128, TB], BF16) for _ in range(2)]
        p.dma(tab[:], rb[:])
        p.dma(J[:], antiI[:])
        p.act(eb[:], tab[:], AF.Exp)
        p.mm(ps[0][:, 0:32], self.ident32[0:32, 31:32].bc([32, 128]), tab[:])
        p.copy(b31[:], ps[0][:, 0:32], eng="act")
        k = 0
        for (src, dst, L) in [(ohS, vS, LS), (ohW, vW, LS), (ohC, vC, LC)]:
            for c0 in range(0, L, TB):
                a, b_ = oh[k % 2], vo[k % 2]
                p.dma(a[:], src[:, c0:c0 + TB])
                p.mm(ps[k % 2][0:32, :], eb[:], a[:])
                p.copy(b_[:], ps[k % 2][0:32, :], eng="act" if k % 2 else "dve")
                p.dma(dst[:, c0:c0 + TB], b_[:])
                k += 1
        geos = [(Ms, vS, LS, OFF_S, [(a, 128 * (a - 3) - 127, 1) for a in range(5)]),
                (Mw, vW, LS, OFF_S, [(a, 128 * (a - 3) - 127, 1) for a in range(8)]),
                (Mc, vC, LC, OFF_C, [(a, 512 * a - (16 * 127 + 31), 16) for a in range(5)])]
        k = 0
        for (M, vec, L, OFF, lst) in geos:
            for (a, base, pstep) in lst:
                for hh in range(32):
                    off = hh * L + OFF + base
                    assert off - hh * L >= 0 and off - hh * L + pstep * 127 + TB - 1 < L, (a, base, L)
                    t_, o_ = tl[k % 2], to[k % 2]
                    p.dma(t_[:], V(vec, bass.AP(vec.t, off, [[pstep, 128], [1, TB]])))
                    p.mm(ps[2 + k % 2][:], J[:], t_[:])
                    p.copy(o_[:], ps[2 + k % 2][:], eng="act" if k % 2 else "dve")
                    p.dma(M[a, hh, :, :], o_[:])
                    k += 1
    p.barrier()
    self.nsa_M = (Ms, Mw, Mc, b31)


Builder.odd_layer = _odd_layer
Builder.nsa_prep = _nsa_prep
_host_inputs_base = host_inputs


def host_inputs(cfg, inp, core, kinds):
    KC = cfg.KC
    m = _host_inputs_base(cfg, inp, core, kinds)
    if "odd" in kinds:
        m.update(nsa_consts(cfg))
        m["rel_bias"] = np.asarray(inp["rel_bias"])
    for l in range(cfg.DEPTH):
        if kinds[l] != "odd":
            continue
        o = l // 2
        w = np.asarray(inp["od_w_in"][o])
        gates = np.zeros((w.shape[0], 128), np.float32)
        gates[:, :96] = w[:, 7168:7264]
        fm = np.concatenate([w[:, 0:4096], w[:, 4096:4608], w[:, 4608:5120], w[:, 5120:5632], w[:, 6144:6656], gates], axis=1)
        tm = np.concatenate([w[:, 5632:6144], w[:, 6656:7168]], axis=1)
        m[f"od_win_fm{l}"] = fm_layout(fm).reshape(49, 128, KC * 128)
        m[f"od_win_tm{l}"] = tm_layout(tm, 256).reshape(4, 128, KC * 256)
        m[f"od_wout{l}"] = fm_layout(np.asarray(inp["od_w_out"][o])).reshape(KC, 128, 32 * 128)
        for i, nm in enumerate(["k", "v"]):
            w1 = np.asarray(inp[f"cmp_{nm}_w1"][o]).reshape(32, 128, 128)
            m[f"cmp_w1_{l}_{i}"] = np.ascontiguousarray(w1.transpose(1, 0, 2))
            m[f"cmp_w2_{l}_{i}"] = np.asarray(inp[f"cmp_{nm}_w2"][o])
            m[f"cmp_pos_{l}_{i}"] = np.ascontiguousarray(np.asarray(inp[f"cmp_{nm}_pos"][o]).T)
    return m
```
